# Optimizing a Trainium2 kernel written in Bass

```python
import jax, jax.numpy as jnp
from jax import lax
import numpy as np

D_MODEL = 2048
BATCH = 1
SEQ = 8192
DEPTH = 4

GRID_W = 64
HEAD_DIM = 128
QBLK = 128
ROPE_THETA = 10000.0
EPS = 1e-6
MLA_HEADS = 6
MLA_Q_RANK = 384
MLA_KV_RANK = 256
MLA_NOPE = 128
MLA_ROPE = 64
MLA_V = 128
GQA_HEADS = 4
GQA_KV_HEADS = 2
NA_HEADS = 6
NA_KH = 8
NA_KW = 16
IN_SIZES = (MLA_Q_RANK, MLA_KV_RANK, MLA_ROPE,
            GQA_HEADS * HEAD_DIM, GQA_KV_HEADS * HEAD_DIM, GQA_KV_HEADS * HEAD_DIM,
            NA_HEADS * HEAD_DIM, NA_HEADS * HEAD_DIM, NA_HEADS * HEAD_DIM)
D_IN = MLA_Q_RANK + MLA_KV_RANK + MLA_ROPE + (GQA_HEADS + 2 * GQA_KV_HEADS) * HEAD_DIM + 3 * NA_HEADS * HEAD_DIM
GROUP_SIZES = (MLA_HEADS * MLA_V, GQA_HEADS * HEAD_DIM, NA_HEADS * HEAD_DIM)
MIX_WIDTH = MLA_HEADS * MLA_V + GQA_HEADS * HEAD_DIM + NA_HEADS * HEAD_DIM
N_GROUPS = 4
EXPERTS_PER_GROUP = 8
N_EXPERTS = N_GROUPS * EXPERTS_PER_GROUP
TOP_K = 2
D_EXPERT = 512
MOE_BLK = 128

kernel_name = 'hybrid_mla_gqa_natten_hmoe_encoder'


def _split_cols(a, sizes):
    out, start = [], 0
    for s in sizes:
        out.append(a[..., start:start + s])
        start += s
    return out


def rms_norm(x, g):
    xf = x.astype(jnp.float32)
    y = xf * lax.rsqrt(jnp.mean(xf * xf, axis=-1, keepdims=True) + EPS)
    return (y * g.astype(jnp.float32)).astype(x.dtype)


def rope(x, pos):
    d = x.shape[-1]
    inv = ROPE_THETA ** (-jnp.arange(0, d, 2, dtype=jnp.float32) / d)
    ang = pos.astype(jnp.float32)[:, None] * inv[None, :]
    cos = jnp.cos(ang)[:, None, :]
    sin = jnp.sin(ang)[:, None, :]
    xf = x.astype(jnp.float32)
    x1, x2 = xf[..., : d // 2], xf[..., d // 2:]
    return jnp.concatenate([x1 * cos - x2 * sin, x2 * cos + x1 * sin], axis=-1).astype(x.dtype)


def axial_rope(x, row, col):
    half = x.shape[-1] // 2
    return jnp.concatenate([rope(x[..., :half], row), rope(x[..., half:], col)], axis=-1)


def sweep_attention(q, k, v):
    B, S, Hk, G, Dq = q.shape
    nb = S // QBLK
    scale = Dq ** -0.5
    qb = jnp.moveaxis(q.reshape(B, nb, QBLK, Hk, G, Dq), 1, 0)

    def block(qblk):
        s = jnp.einsum('bqhgd,bkhd->bhgqk', qblk, k).astype(jnp.float32) * scale
        p = jax.nn.softmax(s, axis=-1).astype(v.dtype)
        return jnp.einsum('bhgqk,bkhd->bqhgd', p, v)

    o = lax.map(block, qb)
    return jnp.moveaxis(o, 0, 1).reshape(B, S, Hk * G * v.shape[-1])


def mla_mixer(a_q, a_kv, a_pe, q_norm_g, w_uq, kv_norm_g, w_ukv, qk_q_g, qk_k_g):
    B, S, _ = a_q.shape
    q = (rms_norm(a_q, q_norm_g) @ w_uq).reshape(B, S, MLA_HEADS, MLA_NOPE + MLA_ROPE)
    kv = (rms_norm(a_kv, kv_norm_g) @ w_ukv).reshape(B, S, MLA_HEADS, MLA_NOPE + MLA_V)
    k_pe = jnp.broadcast_to(a_pe[:, :, None, :], (B, S, MLA_HEADS, MLA_ROPE))
    k = jnp.concatenate([kv[..., :MLA_NOPE], k_pe], axis=-1)
    v = kv[..., MLA_NOPE:]
    q = rms_norm(q, qk_q_g)
    k = rms_norm(k, qk_k_g)
    pos = jnp.arange(S)
    q = jnp.concatenate([q[..., :MLA_NOPE], rope(q[..., MLA_NOPE:], pos)], axis=-1)
    k = jnp.concatenate([k[..., :MLA_NOPE], rope(k[..., MLA_NOPE:], pos)], axis=-1)
    return sweep_attention(q[:, :, :, None, :], k, v)


def gqa_mixer(q, k, v, q_g, k_g):
    B, S, _ = q.shape
    q = rms_norm(q.reshape(B, S, GQA_HEADS, HEAD_DIM), q_g)
    k = rms_norm(k.reshape(B, S, GQA_KV_HEADS, HEAD_DIM), k_g)
    v = v.reshape(B, S, GQA_KV_HEADS, HEAD_DIM)
    t = jnp.arange(S)
    row, col = t // GRID_W, t % GRID_W
    q = axial_rope(q, row, col)
    k = axial_rope(k, row, col)
    q = q.reshape(B, S, GQA_KV_HEADS, GQA_HEADS // GQA_KV_HEADS, HEAD_DIM)
    return sweep_attention(q, k, v)


def neighbourhood_mixer(q, k, v, q_g, k_g, rpb):
    B, S, _ = q.shape
    rows = S // GRID_W
    kh = min(NA_KH, rows)
    q = rms_norm(q.reshape(B, S, NA_HEADS, HEAD_DIM), q_g)
    k = rms_norm(k.reshape(B, S, NA_HEADS, HEAD_DIM), k_g)
    v = v.reshape(B, S, NA_HEADS, HEAD_DIM)
    r = jnp.arange(rows)
    c = jnp.arange(GRID_W)
    rs = jnp.clip(r - kh // 2, 0, rows - kh)
    cs = jnp.clip(c - NA_KW // 2, 0, GRID_W - NA_KW)
    row_idx = rs[:, None] + jnp.arange(kh)[None, :]
    K = kh * GRID_W
    kg = k.reshape(B, rows, GRID_W, NA_HEADS, HEAD_DIM)[:, row_idx].reshape(B, rows, K, NA_HEADS, HEAD_DIM)
    vg = v.reshape(B, rows, GRID_W, NA_HEADS, HEAD_DIM)[:, row_idx].reshape(B, rows, K, NA_HEADS, HEAD_DIM)
    qg = q.reshape(B, rows, GRID_W, NA_HEADS, HEAD_DIM)
    s = jnp.einsum('brqhd,brkhd->bhrqk', qg, kg).astype(jnp.float32) * (HEAD_DIM ** -0.5)
    kr = jnp.repeat(row_idx, GRID_W, axis=1)
    kc = jnp.tile(jnp.arange(GRID_W), kh)
    in_win = (kc[None, :] >= cs[:, None]) & (kc[None, :] < cs[:, None] + NA_KW)
    dr_i = (kr - r[:, None]) + NA_KH - 1
    dc_i = jnp.clip(kc[None, :] - c[:, None] + NA_KW - 1, 0, 2 * NA_KW - 2)
    bias = rpb[:, dr_i[:, None, :], dc_i[None, :, :]].astype(jnp.float32)
    s = jnp.where(in_win, s + bias[None], jnp.finfo(jnp.float32).min)
    p = jax.nn.softmax(s, axis=-1).astype(v.dtype)
    o = jnp.einsum('bhrqk,brkhd->brqhd', p, vg)
    return o.reshape(B, S, NA_HEADS * HEAD_DIM)


def group_rms(o, g):
    parts = _split_cols(o, GROUP_SIZES)
    gparts = _split_cols(g, GROUP_SIZES)
    return jnp.concatenate([rms_norm(p, gp) for p, gp in zip(parts, gparts)], axis=-1)


def hier_moe(h, w_rg, b_rg, w_re, b_re, w_gate, w_up, w_down):
    B, S, D = h.shape
    N = B * S
    xt = h.reshape(N, D)
    g_prob = jax.nn.softmax((xt @ w_rg).astype(jnp.float32) + b_rg.astype(jnp.float32), axis=-1)
    g_top = jnp.argmax(g_prob, axis=-1)
    p_g = jnp.take_along_axis(g_prob, g_top[:, None], axis=-1)
    e_logits = ((xt @ w_re).astype(jnp.float32) + b_re.astype(jnp.float32)).reshape(N, N_GROUPS, EXPERTS_PER_GROUP)
    e_in = jnp.take_along_axis(e_logits, g_top[:, None, None], axis=1)[:, 0]
    e_prob = jax.nn.softmax(e_in, axis=-1)
    w_top, i_top = lax.top_k(e_prob, TOP_K)
    w_top = w_top / jnp.sum(w_top, axis=-1, keepdims=True) * p_g
    eid = (g_top[:, None] * EXPERTS_PER_GROUP + i_top).reshape(-1)
    wts = w_top.reshape(-1)
    A = N * TOP_K
    tok = jnp.repeat(jnp.arange(N), TOP_K)
    order = jnp.argsort(eid)
    se, stok, sw = eid[order], tok[order], wts[order]
    counts = jnp.bincount(eid, length=N_EXPERTS)
    padded = ((counts + MOE_BLK - 1) // MOE_BLK) * MOE_BLK
    pad_end = jnp.cumsum(padded)
    pad_start = pad_end - padded
    start = jnp.cumsum(counts) - counts
    dest = pad_start[se] + jnp.arange(A) - start[se]
    nb = -(-(A + N_EXPERTS * (MOE_BLK - 1)) // MOE_BLK)
    P = nb * MOE_BLK
    xbuf = jnp.zeros((P, D), h.dtype).at[dest].set(xt[stok])
    blk_e = jnp.minimum(jnp.searchsorted(pad_end, jnp.arange(nb) * MOE_BLK, side='right'), N_EXPERTS - 1)

    def expert_block(args):
        xb, e = args
        return (jax.nn.silu(xb @ w_gate[e]) * (xb @ w_up[e])) @ w_down[e]

    ybuf = lax.map(expert_block, (xbuf.reshape(nb, MOE_BLK, D), blk_e)).reshape(P, D)
    y = jnp.zeros((N, D), h.dtype).at[stok].add(ybuf[dest] * sw[:, None].astype(h.dtype))
    return y.reshape(B, S, D)


def setup_inputs(seed: int = 0) -> dict:
    key = jax.random.key(seed)
    ks = iter(jax.random.split(key, 32))

    def nrm(shape, scale):
        return jax.random.normal(next(ks), shape, jnp.float32) * scale

    def gain(shape):
        return 1.0 + nrm(shape, 0.02)

    L, D = DEPTH, D_MODEL
    return {
        'x': nrm((BATCH, SEQ, D), 1.0),
        'c': nrm((BATCH, D), 1.0),
        'ada_w': nrm((L, D, 6 * D), 0.5 * D ** -0.5),
        'ada_b': nrm((L, 6 * D), 0.02),
        'norm1_g': gain((L, D)),
        'norm2_g': gain((L, D)),
        'w_in': nrm((L, D, D_IN), D ** -0.5),
        'mla_q_norm_g': gain((L, MLA_Q_RANK)),
        'mla_w_uq': nrm((L, MLA_Q_RANK, MLA_HEADS * (MLA_NOPE + MLA_ROPE)), MLA_Q_RANK ** -0.5),
        'mla_kv_norm_g': gain((L, MLA_KV_RANK)),
        'mla_w_ukv': nrm((L, MLA_KV_RANK, MLA_HEADS * (MLA_NOPE + MLA_V)), MLA_KV_RANK ** -0.5),
        'mla_qk_q_g': gain((L, MLA_NOPE + MLA_ROPE)),
        'mla_qk_k_g': gain((L, MLA_NOPE + MLA_ROPE)),
        'gqa_q_g': gain((L, HEAD_DIM)),
        'gqa_k_g': gain((L, HEAD_DIM)),
        'na_q_g': gain((L, HEAD_DIM)),
        'na_k_g': gain((L, HEAD_DIM)),
        'na_rpb': nrm((L, NA_HEADS, 2 * NA_KH - 1, 2 * NA_KW - 1), 0.1),
        'mix_out_norm_g': gain((L, MIX_WIDTH)),
        'w_out': nrm((L, MIX_WIDTH, D), MIX_WIDTH ** -0.5),
        'router_group_w': nrm((L, D, N_GROUPS), D ** -0.5),
        'router_group_b': nrm((L, N_GROUPS), 0.01),
        'router_expert_w': nrm((L, D, N_EXPERTS), D ** -0.5),
        'router_expert_b': nrm((L, N_EXPERTS), 0.01),
        'expert_w_gate': nrm((L, N_EXPERTS, D, D_EXPERT), D ** -0.5),
        'expert_w_up': nrm((L, N_EXPERTS, D, D_EXPERT), D ** -0.5),
        'expert_w_down': nrm((L, N_EXPERTS, D_EXPERT, D), D_EXPERT ** -0.5),
    }


def reference(x, c, ada_w, ada_b, norm1_g, norm2_g, w_in, mla_q_norm_g, mla_w_uq, mla_kv_norm_g,
              mla_w_ukv, mla_qk_q_g, mla_qk_k_g, gqa_q_g, gqa_k_g, na_q_g, na_k_g, na_rpb,
              mix_out_norm_g, w_out, router_group_w, router_group_b, router_expert_w,
              router_expert_b, expert_w_gate, expert_w_up, expert_w_down):
    cond = jax.nn.silu(c)
    for l in range(DEPTH):
        mod = cond @ ada_w[l] + ada_b[l]
        sh1, sc1, g1, sh2, sc2, g2 = [m[:, None, :] for m in jnp.split(mod, 6, axis=-1)]
        h = rms_norm(x, norm1_g[l]) * (1 + sc1) + sh1
        a_q, a_kv, a_pe, gq, gk, gv, nq, nk, nv = _split_cols(h @ w_in[l], IN_SIZES)
        o_a = mla_mixer(a_q, a_kv, a_pe, mla_q_norm_g[l], mla_w_uq[l], mla_kv_norm_g[l],
                        mla_w_ukv[l], mla_qk_q_g[l], mla_qk_k_g[l])
        o_b = gqa_mixer(gq, gk, gv, gqa_q_g[l], gqa_k_g[l])
        o_c = neighbourhood_mixer(nq, nk, nv, na_q_g[l], na_k_g[l], na_rpb[l])
        o = group_rms(jnp.concatenate([o_a, o_b, o_c], axis=-1), mix_out_norm_g[l])
        x = x + g1 * (o @ w_out[l])
        h = rms_norm(x, norm2_g[l]) * (1 + sc2) + sh2
        x = x + g2 * hier_moe(h, router_group_w[l], router_group_b[l], router_expert_w[l],
                              router_expert_b[l], expert_w_gate[l], expert_w_up[l], expert_w_down[l])
    return x
```

```python
import numpy as np
from contextlib import ExitStack
import ml_dtypes
import concourse.bass as bass
import concourse.mybir as mybir
from concourse.bass_utils import run_bass_kernel_spmd

F32 = mybir.dt.float32
BF16 = mybir.dt.bfloat16
I32 = mybir.dt.int32
U32 = mybir.dt.uint32
AF = mybir.ActivationFunctionType
ALU = mybir.AluOpType
AX = mybir.AxisListType
NPBF = ml_dtypes.bfloat16

NCORES = 8
D = 2048
SEQ = 8192
TOK = SEQ // NCORES
NTC = TOK // 128
DEPTH = 4
KD = D // 128
EPS = 1e-6
D_IN = 4032
NG, EPG, NE, DE = 4, 8, 32, 512

EPOCH = 20000


class T:
    __slots__ = ("name", "w", "r")

    def __init__(self, name=""):
        self.name = name
        self.w = None
        self.r = []


class DmaSem:
    def __init__(self, prog, name):
        self.sem = prog.new_sem(name)
        self.count = 0


class Op:
    __slots__ = ("eng", "fn", "deps", "need_inc", "semref", "is_dma", "idx", "kind", "flag")

    def __init__(self, eng, fn, is_dma=False, kind="op", flag=None):
        self.eng = eng
        self.fn = fn
        self.deps = []
        self.need_inc = False
        self.semref = None
        self.is_dma = is_dma
        self.kind = kind
        self.flag = flag


class Prog:
    ENGS = ("pe", "act", "dve", "pool", "sp")

    def __init__(self, nc, stack):
        self.nc = nc
        self.ops = {e: [] for e in self.ENGS}
        self.n = 0
        self._stack = stack
        self.nsem = 0
        self.final_dsems = []

    def new_sem(self, name):
        self.nsem += 1
        return self._stack.enter_context(self.nc.semaphore(f"{name}_{self.nsem}"))

    def dsem(self, name):
        return DmaSem(self, name)

    def _record(self, op, reads, writes):
        deps = []
        for t in reads:
            if t.w is not None:
                deps.append(t.w)
        for t in writes:
            if t.w is not None:
                deps.append(t.w)
            deps.extend(t.r)
        seen = set()
        for d in deps:
            if id(d) in seen or d is op:
                continue
            seen.add(id(d))
            if d.eng == "pe" and op.eng == "pe" and not d.is_dma and not op.is_dma:
                continue
            op.deps.append(d)
            d.need_inc = True
        for t in reads:
            t.r.append(op)
        for t in writes:
            t.w = op
            t.r = []
        op.idx = self.n
        self.n += 1
        self.ops[op.eng].append(op)
        return op

    def op(self, eng, fn, reads=(), writes=()):
        return self._record(Op(eng, fn), reads, writes)

    def cond_begin(self, flag_ap, tflag):
        for e in ("pe", "act", "dve", "pool"):
            self._record(Op(e, None, kind="cb", flag=flag_ap), [tflag], [])

    def cond_end(self):
        for e in ("pe", "act", "dve", "pool"):
            self._record(Op(e, None, kind="ce"), [], [])

    def dma(self, eng, out, in_, dsem, reads=(), writes=(), **kw):
        def fn(e, out=out, in_=in_, kw=kw):
            return e.dma_start(out=out, in_=in_, **kw)
        op = Op(eng, fn, is_dma=True)
        dsem.count += 16
        op.semref = (dsem.sem, dsem.count)
        return self._record(op, reads, writes)

    def emit(self, block):
        for e in self.ENGS:
            cnt = 0
            sem = None
            for op in self.ops[e]:
                if op.is_dma or op.kind != "op":
                    continue
                if op.need_inc:
                    if sem is None or cnt >= EPOCH:
                        sem = self.new_sem(f"p_{e}")
                        cnt = 0
                    cnt += 1
                    op.semref = (sem, cnt)

        def run(e_name, eng):
            waited = {}
            region = None
            creg = [None]
            ops = self.ops[e_name]
            skip_ce = False
            for oi, op in enumerate(ops):
                if op.kind == "ce":
                    if skip_ce:
                        skip_ce = False
                        continue
                    region["guard"].__exit__(None, None, None)
                    if region["incs"]:
                        eg = eng.Else()
                        eg.__enter__()
                        for (sm, n) in region["incs"].values():
                            eng.drain().then_inc(sm, n)
                        eg.__exit__(None, None, None)
                    waited = region["waited"]
                    region = None
                    continue
                if op.kind == "cb" and ops[oi + 1].kind == "ce":
                    skip_ce = True
                    continue
                need = {}
                for d in op.deps:
                    s, v = d.semref
                    k = id(s)
                    if waited.get(k, 0) >= v:
                        continue
                    if k not in need or need[k][1] < v:
                        need[k] = (s, v)
                for k, (s, v) in need.items():
                    eng.wait_ge(s, v)
                    waited[k] = v
                if op.kind == "cb":
                    if creg[0] is None:
                        creg[0] = eng.alloc_register(f"creg_{e_name}")
                    eng.load(creg[0], op.flag)
                    g = eng.If_ne(creg[0], 0)
                    g.__enter__()
                    region = dict(guard=g, waited=dict(waited), incs={})
                    continue
                ins = op.fn(eng)
                if op.is_dma:
                    assert region is None
                    ins.then_inc(op.semref[0], 16)
                elif op.need_inc:
                    ins.then_inc(op.semref[0], 1)
                    if region is not None:
                        k = id(op.semref[0])
                        region["incs"][k] = (op.semref[0], region["incs"].get(k, (None, 0))[1] + 1)

        @block.tensor
        def _(eng):
            run("pe", eng)

        @block.scalar
        def _(eng):
            run("act", eng)

        @block.vector
        def _(eng):
            run("dve", eng)

        @block.gpsimd
        def _(eng):
            run("pool", eng)

        @block.sync
        def _(eng):
            run("sp", eng)
            for ds in self.final_dsems:
                eng.wait_ge(ds.sem, ds.count)


class Ctx:
    def __init__(self):
        self.nc = bass.Bass("TRN2", target_bir_lowering=False)
        self.st = ExitStack()
        self.P = Prog(self.nc, self.st)
        self.banks = None
        self.n = 0

    def sb(self, name, shape, dt):
        return self.st.enter_context(self.nc.sbuf_tensor("sb_" + name, shape, dt))

    def din(self, name, shape, dt):
        return self.nc.dram_tensor(name, list(shape), dt, kind="ExternalInput").ap()

    def dout(self, name, shape, dt):
        return self.nc.dram_tensor(name, list(shape), dt, kind="ExternalOutput").ap()

    def psum_banks(self):
        self.banks = [self.st.enter_context(self.nc.psum_tensor(f"bank{i}", [128, 512], F32)) for i in range(8)]
        self.bankT = [T(f"bank{i}") for i in range(8)]

    def ident(self):
        P = self.P
        io = self.sb("ident_io", [128, 128], I32)
        idb = self.sb("ident_bf", [128, 128], BF16)
        idf = self.sb("ident_f", [128, 128], F32)
        tio, tid = T("io"), T("ident")
        P.op("pool", lambda e: e.iota(io[:], pattern=[[1, 128]], base=0, channel_multiplier=-1), writes=[tio])
        P.op("dve", lambda e: e.tensor_single_scalar(idb[:], io[:], 0, ALU.is_equal), reads=[tio], writes=[tid])
        P.op("dve", lambda e: e.tensor_single_scalar(idf[:], io[:], 0, ALU.is_equal), reads=[tio], writes=[tid])
        self.idb, self.idf, self.tid = idb, idf, tid

    def finish(self):
        block = self.st.enter_context(self.nc.Block())
        self.P.emit(block)
        self.st.close()
        return self.nc


class Ring:
    def __init__(self, cx, name, shape, dt, n, dma=True):
        self.bufs = [cx.sb(f"{name}{i}", shape, dt) for i in range(n)]
        self.ts = [T(f"{name}{i}") for i in range(n)]
        self.ds = [cx.P.dsem(f"s_{name}{i}") if dma else None for i in range(n)]
        self.i = 0
        self.n = n

    def next(self):
        i = self.i % self.n
        self.i += 1
        return self.bufs[i], self.ts[i], self.ds[i]

    def skip(self):
        self.i += 1


MODC = 6 * D // NCORES


def build_ada():
    cx = Ctx()
    P = cx.P
    cT = cx.din("cT", [128, KD], F32)
    w = cx.din("w", [DEPTH, D, MODC], F32)
    b = cx.din("b", [1, DEPTH * MODC], F32)
    o = cx.dout("o", [1, DEPTH * MODC], F32)
    cx.psum_banks()
    c_sb = cx.sb("c_sb", [128, KD], F32)
    cond = cx.sb("cond", [128, KD], F32)
    b_sb = cx.sb("b_sb", [1, DEPTH * MODC], F32)
    o_sb = cx.sb("o_sb", [1, DEPTH * MODC], F32)
    tc_, tcond, tb, to = T(), T(), T(), T()
    s0 = P.dsem("s0")
    so = P.dsem("so")
    P.dma("sp", c_sb[:], cT, s0, writes=[tc_])
    P.dma("sp", b_sb[:], b, s0, writes=[tb])
    P.op("act", lambda e: e.activation(cond[:], c_sb[:], AF.Silu), reads=[tc_], writes=[tcond])
    wr = Ring(cx, "w", [128, KD, 512], F32, 3)
    i = 0
    for l in range(DEPTH):
        for cb in range(MODC // 512):
            wt, wT, ws = wr.next()
            P.dma("sp" if i % 2 == 0 else "act", wt[:], w[l, :, cb * 512:(cb + 1) * 512].rearrange("(k p) n -> p k n", p=128),
                  ws, writes=[wT])
            bk = i % 2
            for k in range(KD):
                P.op("pe", lambda e, k=k, wt=wt, bk=bk: e.matmul(cx.banks[bk][0:1, :], cond[:, k:k + 1], wt[:, k, :],
                                                                  start=(k == 0), stop=(k == KD - 1)),
                     reads=[tcond, wT], writes=[cx.bankT[bk]])
            c0 = l * MODC + cb * 512
            P.op("dve", lambda e, bk=bk, c0=c0: e.tensor_tensor(o_sb[0:1, c0:c0 + 512], cx.banks[bk][0:1, :],
                                                                  b_sb[0:1, c0:c0 + 512], ALU.add),
                 reads=[cx.bankT[bk], tb], writes=[to])
            i += 1
    P.dma("sp", o, o_sb[:], so, reads=[to])
    P.final_dsems = [so]
    return cx.finish()


QKV_COLS = 6400
OFF = dict(qm=0, km=1152, vm=2304, qg=3072, kg=3584, vg=3840, qn=4096, kn=4864, vn=5632)
NVT = 53
NGB = 896


class Stat:
    def __init__(self, cx, name, n):
        self.t = cx.sb(name, [128, n], F32)
        self.o = 0
        self.n = n

    def get(self, w):
        assert self.o + w <= self.n
        ap = self.t[:, self.o:self.o + w]
        self.o += w
        return ap, T()


def bc(ap, shape):
    return ap.to_broadcast(list(shape))


def build_pre():
    cx = Ctx()
    P = cx.P
    x = cx.din("x", [TOK, D], F32)
    vT_d = cx.din("vT", [128, NVT], F32)
    gb_d = cx.din("gb", [1, NGB], F32)
    csm_d = cx.din("csm", [TOK, 64], F32)
    csa_d = cx.din("csa", [TOK, 128], F32)
    w_in = cx.din("w_in", [D, D_IN], F32)
    w_uq = cx.din("w_uq", [384, 1152], F32)
    w_ukv = cx.din("w_ukv", [256, 1536], F32)
    qkv = cx.dout("qkv", [TOK, QKV_COLS], BF16)
    cx.psum_banks()
    cx.ident()
    banks, bankT = cx.banks, cx.bankT
    st = Stat(cx, "stat", 1024)

    vT = cx.sb("vT", [128, NVT], F32)
    gb = cx.sb("gb", [128, NGB], F32)
    csm = cx.sb("csm", [128, NTC, 64], F32)
    csa = cx.sb("csa", [128, NTC, 128], F32)
    mhalf = cx.sb("mhalf", [128, 8], F32)
    tv, tgb, tcsm, tcsa, tmh = T(), T(), T(), T(), T()
    s0 = P.dsem("s0")
    P.dma("sp", vT[:], vT_d, s0, writes=[tv])
    P.dma("sp", gb[:], gb_d.to_broadcast([128, NGB]), s0, writes=[tgb])
    P.dma("sp", csm[:], csm_d.rearrange("(c p) n -> p c n", p=128), s0, writes=[tcsm])
    P.dma("sp", csa[:], csa_d.rearrange("(c p) n -> p c n", p=128), s0, writes=[tcsa])
    P.op("pool", lambda e: e.memset(mhalf[:], -0.5), writes=[tmh])
    A1T, tA1 = st.get(KD)
    P.op("dve", lambda e: e.scalar_tensor_tensor(A1T, vT[:, 16:32], 1.0, vT[:, 0:16], ALU.add, ALU.mult),
         reads=[tv], writes=[tA1])

    def rstd_of(ss, tss, w, div):
        ms, tms = st.get(w)
        rs, trs = st.get(w)
        P.op("pool", lambda e: e.tensor_scalar(ms, ss, 1.0 / div, EPS, ALU.mult, ALU.add), reads=[tss], writes=[tms])
        P.op("pool", lambda e: e.tensor_tensor(rs, ms, mhalf[:, 0:w], ALU.pow), reads=[tms, tmh], writes=[trs])
        return rs, trs

    hn = cx.sb("hn", [128, NTC, D], BF16)
    thn = [T() for _ in range(NTC)]
    xr = Ring(cx, "x", [128, D], F32, 2)
    junk = cx.sb("junk", [128, D], BF16)
    tjunk = T()
    for tc in range(NTC):
        xt, xT, xs = xr.next()
        P.dma("sp", xt[:], x[tc * 128:(tc + 1) * 128, :], xs, writes=[xT])
        ss, tss = st.get(1)
        P.op("act", lambda e, xt=xt, ss=ss: e.activation(junk[:], xt[:], AF.Square, accum_out=ss),
             reads=[xT], writes=[tjunk, tss])
        rs, trs = rstd_of(ss, tss, 1, D)
        P.op("dve", lambda e, xt=xt, rs=rs, tc=tc: e.tensor_scalar(hn[:, tc, :], xt[:], rs, None, ALU.mult),
             reads=[xT, trs], writes=[thn[tc]])

    h1T = cx.sb("h1T", [128, KD, TOK], BF16)
    th1T = T()
    for k in range(KD):
        bk = 6 + (k % 2)
        psv = banks[bk][:].bitcast(BF16)
        for tc in range(NTC):
            P.op("pe", lambda e, psv=psv, tc=tc, k=k: e.transpose(psv[:, tc * 128:(tc + 1) * 128],
                                                                   hn[:, tc, k * 128:(k + 1) * 128], cx.idb[:]),
                 reads=[thn[tc], cx.tid], writes=[bankT[bk]])
        P.op("act", lambda e, psv=psv, k=k: e.activation(h1T[:, k, :], psv, AF.Identity,
                                                          scale=A1T[:, k:k + 1], bias=vT[:, 32 + k:33 + k]),
             reads=[bankT[bk], tA1, tv], writes=[th1T])

    rawr = Ring(cx, "raw", [128, 1536], F32, 2, dma=False)
    sqr = Ring(cx, "sq", [128, 1536], F32, 2, dma=False)
    yr = Ring(cx, "y", [128, 1152], F32, 2, dma=False)
    tr_ = Ring(cx, "tmp", [128, 4, 256], F32, 2, dma=False)
    tmpTs = {}
    outr = Ring(cx, "ost", [128, 1152], BF16, 4)

    def evac(bl, want_sq=True):
        raw, rT, _ = rawr.next()
        sq, sT, _ = sqr.next()
        c = 0
        for bk, n in bl:
            P.op("act", lambda e, raw=raw, c=c, n=n, bk=bk: e.copy(raw[:, c:c + n], banks[bk][:, 0:n]),
                 reads=[bankT[bk]], writes=[rT])
            if want_sq:
                P.op("act", lambda e, sq=sq, c=c, n=n, bk=bk: e.activation(sq[:, c:c + n], banks[bk][:, 0:n], AF.Square),
                     reads=[bankT[bk]], writes=[sT])
            c += n
        return raw, rT, sq, sT

    def store(tc, ost, oT, osem, col0, n):
        P.dma("sp", qkv[tc * 128:(tc + 1) * 128, col0:col0 + n], ost[:, 0:n], osem, reads=[oT])

    def rope(yv, tY, ov, tO, cs, tcs, H, A):
        y5 = yv.rearrange("p h (a two d) -> p h a two d", a=A, two=2)
        o5 = ov.rearrange("p h (a two d) -> p h a two d", a=A, two=2)
        x1, x2 = y5[:, :, :, 0, :], y5[:, :, :, 1, :]
        shp = [128, H, A, 32]
        cos = bc(cs[:, 0:A * 32].rearrange("p (a d) -> p a d", a=A).unsqueeze(1), shp)
        sin = bc(cs[:, A * 32:2 * A * 32].rearrange("p (a d) -> p a d", a=A).unsqueeze(1), shp)
        tmp, tT, _ = tr_.next()
        n = H * A * 32
        tv_ = [tmp[:, i, 0:n].rearrange("p (h a d) -> p h a d", h=H, a=A) for i in range(4)]
        tTs = tmpTs.setdefault(id(tT), [T() for _ in range(4)])
        P.op("dve", lambda e: e.tensor_tensor(tv_[0], x1, cos, ALU.mult), reads=[tY, tcs], writes=[tTs[0]])
        P.op("dve", lambda e: e.tensor_tensor(tv_[1], x2, sin, ALU.mult), reads=[tY, tcs], writes=[tTs[1]])
        P.op("pool", lambda e: e.tensor_tensor(tv_[2], x2, cos, ALU.mult), reads=[tY, tcs], writes=[tTs[2]])
        P.op("pool", lambda e: e.tensor_tensor(tv_[3], x1, sin, ALU.mult), reads=[tY, tcs], writes=[tTs[3]])
        P.op("dve", lambda e: e.tensor_tensor(o5[:, :, :, 0, :], tv_[0], tv_[1], ALU.subtract),
             reads=[tTs[0], tTs[1]], writes=[tO])
        P.op("pool", lambda e: e.tensor_tensor(o5[:, :, :, 1, :], tv_[2], tv_[3], ALU.add),
             reads=[tTs[2], tTs[3]], writes=[tO])

    def heads_norm(raw3, rT, sq3, sT, H, Dh, g_ap, extra=None, div=None):
        ss, tss = st.get(H)
        P.op("dve", lambda e: e.tensor_reduce(ss, sq3, AX.X, ALU.add), reads=[sT], writes=[tss])
        if extra is not None:
            ex, tex = extra
            P.op("dve", lambda e: e.tensor_tensor(ss, ss, bc(ex, [128, H]), ALU.add), reads=[tss, tex], writes=[tss])
        rs, trs = rstd_of(ss, tss, H, div or Dh)
        y, yT, _ = yr.next()
        y3 = y[:, 0:H * Dh].rearrange("p (h d) -> p h d", h=H)
        P.op("dve", lambda e: e.tensor_tensor(y3, raw3, bc(rs.unsqueeze(2), [128, H, Dh]), ALU.mult),
             reads=[rT, trs], writes=[yT])
        return y3, yT, rs, trs

    aqn = cx.sb("aqn", [128, NTC, 384], BF16)
    akvn = cx.sb("akvn", [128, NTC, 256], BF16)
    kper = cx.sb("kper", [128, NTC, 64], F32)
    sspe, _ = st.get(NTC)
    taqn = [T() for _ in range(NTC)]
    takvn = [T() for _ in range(NTC)]
    tkper = [T() for _ in range(NTC)]
    tsspe = [T() for _ in range(NTC)]

    G_QQ, G_QK, G_GQ, G_GK, G_NQ, G_NK = 0, 192, 384, 512, 640, 768

    blocks = [(0, 704, [384, 320]), (704, 512, [512]), (1216, 512, [512]),
              (1728, 768, [384, 384]), (2496, 768, [384, 384]), (3264, 768, [384, 384])]
    win1 = cx.sb("win1", [128, KD, 768], BF16)
    win0 = hn[:].rearrange("p c d -> p (c d)")[:, 0:KD * 768].rearrange("p (k n) -> p k n", k=KD)
    wslots = [(win0, T(), P.dsem("s_win0")), (win1[:], T(), P.dsem("s_win1"))]
    it = 0
    for b, (c0, n, subs) in enumerate(blocks):
        wt, wT, ws = wslots[(b + 1) % 2]
        for kk in range(0, KD, 4):
            P.dma("pool", wt[:, kk:kk + 4, 0:n], w_in[kk * 128:(kk + 4) * 128, c0:c0 + n].rearrange("(k p) n -> p k n", p=128),
                  ws, writes=[wT] + (thn if (b + 1) % 2 == 0 else []))
        for tc in range(NTC):
            bset = (it % 2) * 3
            it += 1
            s0_ = 0
            bl = []
            for si, sn in enumerate(subs):
                bk = bset + si
                for k in range(KD):
                    P.op("pe", lambda e, bk=bk, sn=sn, k=k, tc=tc, wt=wt, s0_=s0_: e.matmul(
                        banks[bk][:, 0:sn], h1T[:, k, tc * 128:(tc + 1) * 128], wt[:, k, s0_:s0_ + sn],
                        start=(k == 0), stop=(k == KD - 1)), reads=[th1T, wT], writes=[bankT[bk]])
                bl.append((bk, sn))
                s0_ += sn
            if b == 0:
                raw, rT, sq, sT = evac(bl)
                ss, tss = st.get(2)
                P.op("dve", lambda e, ss=ss, sq=sq: e.tensor_reduce(ss[:, 0:1], sq[:, 0:384], AX.X, ALU.add), reads=[sT], writes=[tss])
                P.op("dve", lambda e, ss=ss, sq=sq: e.tensor_reduce(ss[:, 1:2], sq[:, 384:640], AX.X, ALU.add), reads=[sT, tss], writes=[tss])
                P.op("dve", lambda e, sq=sq, tc=tc: e.tensor_reduce(sspe[:, tc:tc + 1], sq[:, 640:704], AX.X, ALU.add),
                     reads=[sT], writes=[tsspe[tc]])
                rq, trq = rstd_of(ss[:, 0:1], tss, 1, 384)
                rk, trk = rstd_of(ss[:, 1:2], tss, 1, 256)
                P.op("dve", lambda e, raw=raw, rq=rq, tc=tc: e.tensor_scalar(aqn[:, tc, :], raw[:, 0:384], rq, None, ALU.mult),
                     reads=[rT, trq], writes=[taqn[tc]])
                P.op("dve", lambda e, raw=raw, rk=rk, tc=tc: e.tensor_scalar(akvn[:, tc, :], raw[:, 384:640], rk, None, ALU.mult),
                     reads=[rT, trk], writes=[takvn[tc]])
                y, yT, _ = yr.next()
                P.op("dve", lambda e, y=y, raw=raw: e.tensor_tensor(y[:, 0:64], raw[:, 640:704], gb[:, G_QK + 128:G_QK + 192], ALU.mult),
                     reads=[rT, tgb], writes=[yT])
                rope(y[:, 0:64].unsqueeze(1), yT, kper[:, tc, :].unsqueeze(1), tkper[tc], csm[:, tc, :], tcsm, 1, 1)
            elif b == 1:
                raw, rT, sq, sT = evac(bl)
                y3, yT, _, _ = heads_norm(raw[:, 0:512].rearrange("p (h d) -> p h d", h=4), rT,
                                          sq[:, 0:512].rearrange("p (h d) -> p h d", h=4), sT, 4, 128, None)
                P.op("pool", lambda e, y3=y3: e.tensor_tensor(y3, y3, bc(gb[:, G_GQ:G_GQ + 128].unsqueeze(1), [128, 4, 128]), ALU.mult),
                     reads=[yT, tgb], writes=[yT])
                ost, oT, osem = outr.next()
                rope(y3, yT, ost[:, 0:512].rearrange("p (h d) -> p h d", h=4), oT, csa[:, tc, :], tcsa, 4, 2)
                store(tc, ost, oT, osem, OFF["qg"], 512)
            elif b == 2:
                raw, rT, sq, sT = evac(bl)
                y3, yT, _, _ = heads_norm(raw[:, 0:256].rearrange("p (h d) -> p h d", h=2), rT,
                                          sq[:, 0:256].rearrange("p (h d) -> p h d", h=2), sT, 2, 128, None)
                P.op("pool", lambda e, y3=y3: e.tensor_tensor(y3, y3, bc(gb[:, G_GK:G_GK + 128].unsqueeze(1), [128, 2, 128]), ALU.mult),
                     reads=[yT, tgb], writes=[yT])
                ost, oT, osem = outr.next()
                rope(y3, yT, ost[:, 0:256].rearrange("p (h d) -> p h d", h=2), oT, csa[:, tc, :], tcsa, 2, 2)
                P.op("act", lambda e, ost=ost, raw=raw: e.copy(ost[:, 256:512], raw[:, 256:512]), reads=[rT], writes=[oT])
                store(tc, ost, oT, osem, OFF["kg"], 512)
            elif b in (3, 4):
                raw, rT, sq, sT = evac(bl)
                y3, yT, _, _ = heads_norm(raw[:, 0:768].rearrange("p (h d) -> p h d", h=6), rT,
                                          sq[:, 0:768].rearrange("p (h d) -> p h d", h=6), sT, 6, 128, None)
                g0 = G_NQ if b == 3 else G_NK
                ost, oT, osem = outr.next()
                P.op("pool", lambda e, y3=y3, ost=ost, g0=g0: e.tensor_tensor(
                    ost[:, 0:768].rearrange("p (h d) -> p h d", h=6), y3,
                    bc(gb[:, g0:g0 + 128].unsqueeze(1), [128, 6, 128]), ALU.mult), reads=[yT, tgb], writes=[oT])
                store(tc, ost, oT, osem, OFF["qn"] if b == 3 else OFF["kn"], 768)
            else:
                ost, oT, osem = outr.next()
                c = 0
                for bk, sn in bl:
                    P.op("act", lambda e, ost=ost, c=c, sn=sn, bk=bk: e.copy(ost[:, c:c + sn], banks[bk][:, 0:sn]),
                         reads=[bankT[bk]], writes=[oT])
                    c += sn
                store(tc, ost, oT, osem, OFF["vn"], 768)

    aqnT = cx.sb("aqnT", [128, 3, TOK], BF16)
    akvnT = cx.sb("akvnT", [128, 2, TOK], BF16)
    taqnT, takvnT = T(), T()
    i = 0
    for (src, tsrc, dst, tdst, nj, vcol) in ((aqn, taqn, aqnT, taqnT, 3, 48), (akvn, takvn, akvnT, takvnT, 2, 51)):
        for j in range(nj):
            bk = 6 + (i % 2)
            i += 1
            psv = banks[bk][:].bitcast(BF16)
            for tc in range(NTC):
                P.op("pe", lambda e, psv=psv, tc=tc, j=j, src=src: e.transpose(
                    psv[:, tc * 128:(tc + 1) * 128], src[:, tc, j * 128:(j + 1) * 128], cx.idb[:]),
                    reads=[tsrc[tc], cx.tid], writes=[bankT[bk]])
            P.op("act", lambda e, psv=psv, j=j, dst=dst, vcol=vcol: e.activation(
                dst[:, j, :], psv, AF.Identity, scale=vT[:, vcol + j:vcol + j + 1]),
                reads=[bankT[bk], tv], writes=[tdst])

    w1flat = win1[:].rearrange("p k n -> p (k n)")
    uq = w1flat[:, 0:3456].rearrange("p (k n) -> p k n", k=3)
    ukv = w1flat[:, 3456:3456 + 3072].rearrange("p (k n) -> p k n", k=2)
    tuq = tukv = wslots[1][1]
    suq = wslots[1][2]
    P.dma("pool", uq, w_uq.rearrange("(k p) n -> p k n", p=128), suq, writes=[tuq])
    P.dma("pool", ukv, w_ukv.rearrange("(k p) n -> p k n", p=128), suq, writes=[tukv])
    for tc in range(NTC):
        bset = (it % 2) * 3
        it += 1
        bl = []
        for si in range(3):
            bk = bset + si
            for k in range(3):
                P.op("pe", lambda e, bk=bk, si=si, k=k, tc=tc: e.matmul(
                    banks[bk][:, 0:384], aqnT[:, k, tc * 128:(tc + 1) * 128], uq[:, k, si * 384:(si + 1) * 384],
                    start=(k == 0), stop=(k == 2)), reads=[taqnT, tuq], writes=[bankT[bk]])
            bl.append((bk, 384))
        raw, rT, sq, sT = evac(bl)
        y3, yT, _, _ = heads_norm(raw[:, 0:1152].rearrange("p (h d) -> p h d", h=6), rT,
                                  sq[:, 0:1152].rearrange("p (h d) -> p h d", h=6), sT, 6, 192, None)
        P.op("pool", lambda e, y3=y3: e.tensor_tensor(y3, y3, bc(gb[:, G_QQ:G_QQ + 192].unsqueeze(1), [128, 6, 192]), ALU.mult),
             reads=[yT, tgb], writes=[yT])
        ost, oT, osem = outr.next()
        o3 = ost[:, 0:1152].rearrange("p (h d) -> p h d", h=6)
        P.op("act", lambda e, o3=o3, y3=y3: e.copy(o3[:, :, 0:128], y3[:, :, 0:128]), reads=[yT], writes=[oT])
        rope(y3[:, :, 128:192], yT, o3[:, :, 128:192], oT, csm[:, tc, :], tcsm, 6, 1)
        store(tc, ost, oT, osem, OFF["qm"], 1152)

    for tc in range(NTC):
        bset = (it % 2) * 3
        it += 1
        bl = []
        for si in range(3):
            bk = bset + si
            for k in range(2):
                P.op("pe", lambda e, bk=bk, si=si, k=k, tc=tc: e.matmul(
                    banks[bk][:, 0:512], akvnT[:, k, tc * 128:(tc + 1) * 128], ukv[:, k, si * 512:(si + 1) * 512],
                    start=(k == 0), stop=(k == 1)), reads=[takvnT, tukv], writes=[bankT[bk]])
            bl.append((bk, 512))
        raw, rT, sq, sT = evac(bl)
        raw3 = raw[:, 0:1536].rearrange("p (h d) -> p h d", h=6)
        sq3 = sq[:, 0:1536].rearrange("p (h d) -> p h d", h=6)
        y3, yT, rs, trs = heads_norm(raw3[:, :, 0:128], rT, sq3[:, :, 0:128], sT, 6, 128, None,
                                     extra=(sspe[:, tc:tc + 1], tsspe[tc]), div=192)
        ost, oT, osem = outr.next()
        o3 = ost[:, 0:1152].rearrange("p (h d) -> p h d", h=6)
        P.op("pool", lambda e, o3=o3, y3=y3: e.tensor_tensor(o3[:, :, 0:128], y3, bc(gb[:, G_QK:G_QK + 128].unsqueeze(1), [128, 6, 128]), ALU.mult),
             reads=[yT, tgb], writes=[oT])
        P.op("dve", lambda e, o3=o3, rs=rs, tc=tc: e.tensor_tensor(
            o3[:, :, 128:192], bc(kper[:, tc, :].unsqueeze(1), [128, 6, 64]), bc(rs.unsqueeze(2), [128, 6, 64]), ALU.mult),
            reads=[tkper[tc], trs], writes=[oT])
        store(tc, ost, oT, osem, OFF["km"], 1152)
        ost2, oT2, osem2 = outr.next()
        P.op("act", lambda e, ost2=ost2, raw3=raw3: e.copy(ost2[:, 0:768].rearrange("p (h d) -> p h d", h=6), raw3[:, :, 128:256]),
             reads=[rT], writes=[oT2])
        store(tc, ost2, oT2, osem2, OFF["vm"], 768)

    P.final_dsems = outr.ds
    return cx.finish()


def colT(v):
    return np.ascontiguousarray(np.asarray(v, np.float32).reshape(-1, 128).T)


def rope_tables():
    inv = (10000.0 ** (-np.arange(0, 64, 2, dtype=np.float32) / np.float32(64))).astype(np.float32)
    t = np.arange(SEQ)
    def cs(pos):
        ang = pos.astype(np.float32)[:, None] * inv[None, :]
        return np.cos(ang).astype(np.float32), np.sin(ang).astype(np.float32)
    cm, sm = cs(t)
    cr, sr = cs(t // 64)
    cc, sc = cs(t % 64)
    csm = np.concatenate([cm, sm], axis=1)
    csa = np.concatenate([cr, cc, sr, sc], axis=1)
    return np.ascontiguousarray(csm), np.ascontiguousarray(csa)


def pre_inputs(inp, l, x_full, mod, tabs):
    csm, csa = tabs
    m = mod[l].reshape(6, D)
    vT = np.concatenate([colT(inp["norm1_g"][l]), colT(m[1]), colT(m[0]),
                         colT(inp["mla_q_norm_g"][l]), colT(inp["mla_kv_norm_g"][l])], axis=1)
    gb = np.concatenate([inp["mla_qk_q_g"][l], inp["mla_qk_k_g"][l], inp["gqa_q_g"][l], inp["gqa_k_g"][l],
                         inp["na_q_g"][l], inp["na_k_g"][l]]).reshape(1, -1).astype(np.float32)
    maps = []
    for j in range(NCORES):
        sl = slice(j * TOK, (j + 1) * TOK)
        maps.append({"x": np.ascontiguousarray(x_full[sl]), "vT": vT, "gb": gb, "csm": csm[sl], "csa": csa[sl],
                     "w_in": inp["w_in"][l], "w_uq": inp["mla_w_uq"][l], "w_ukv": inp["mla_w_ukv"][l]})
    return maps


NPIECE = 4
KPB = 16
NHALO = 14
NBT = 29
NEG = -30000.0


def na_slots(p):
    d = [-2, -1, 0, 1, 2]
    if p == 0:
        d.append(3)
    if p == 7:
        d.append(-3)
    return d


def na_tile_index(p, si):
    if 2 <= p <= 5:
        return si
    sp = {0: 0, 1: 1, 6: 2, 7: 3}[p]
    return 5 + sp * 6 + si


def build_attn():
    cx = Ctx()
    P = cx.P
    qa = cx.din("qa", [10, 128, TOK], BF16)
    qb_ = cx.din("qb", [6, 64, TOK], BF16)
    ka = cx.din("ka", [10, 128, SEQ], BF16)
    kb_ = cx.din("kb", [6, 64, SEQ], BF16)
    va = cx.din("va", [10, NPIECE, 128, KPB * 130], BF16)
    qn = cx.din("qn", [6, 128, TOK], BF16)
    kn = cx.din("kn", [6, 128, NHALO * 128], BF16)
    vn = cx.din("vn", [6, 128, NHALO * 130], BF16)
    bn = cx.din("bn", [6, 128, NBT * 128], F32)
    o = cx.dout("o", [TOK, D], F32)
    cx.psum_banks()
    banks, bankT = cx.banks, cx.bankT

    qr = Ring(cx, "q", [128, 2, TOK], BF16, 2)
    kr = Ring(cx, "k", [128, 2, KPB * 128], BF16, 3)
    vr = Ring(cx, "v", [128, KPB * 130], BF16, 3)
    ptr = Ring(cx, "pt", [128, 512], BF16, 4, dma=False)
    osr = Ring(cx, "os", [128, 8, 128], F32, 2)
    rinv = cx.sb("rinv", [128, 256], F32)
    rcnt = [0]
    o_v = o.rearrange("(a p) c -> p a c", p=128)

    def finalize(bk, col, ost, oT, a):
        ri = rinv[:, rcnt[0] % 256:rcnt[0] % 256 + 1]
        rcnt[0] += 1
        tri = T()
        P.op("dve", lambda e: e.reciprocal(ri, banks[bk][:, col + 128:col + 129]), reads=[bankT[bk]], writes=[tri])
        P.op("dve", lambda e: e.tensor_scalar(ost[:, a, :], banks[bk][:, col:col + 128], ri, None, ALU.mult),
             reads=[bankT[bk], tri], writes=[oT])

    sbank = [0]
    for hd in range(10):
        mla = hd < 6
        scale = (192.0 if mla else 128.0) ** -0.5
        qt, qT, qs = qr.next()
        P.dma("sp", qt[:, 0, :], qa[hd], qs, writes=[qT])
        if mla:
            P.dma("sp", qt[0:64, 1, :], qb_[hd], qs, writes=[qT])
        ost, oT, osem = osr.next()
        for pc in range(NPIECE):
            kt, kT, ks = kr.next()
            vt, vT_, vs = vr.next()
            P.dma("sp", kt[:, 0, :], ka[hd, :, pc * 2048:(pc + 1) * 2048], ks, writes=[kT])
            if mla:
                P.dma("sp", kt[0:64, 1, :], kb_[hd, :, pc * 2048:(pc + 1) * 2048], ks, writes=[kT])
            P.dma("pool", vt[:], va[hd, pc], vs, writes=[vT_])
            for kb in range(KPB):
                for qb in range(2):
                    sb_ = 4 + (sbank[0] % 4)
                    sbank[0] += 1
                    P.op("pe", lambda e, sb_=sb_, kt=kt, qt=qt, kb=kb, qb=qb, mla=mla: e.matmul(
                        banks[sb_][:, :], kt[:, 0, kb * 128:(kb + 1) * 128], qt[:, 0, qb * 512:(qb + 1) * 512],
                        start=True, stop=not mla), reads=[kT, qT], writes=[bankT[sb_]])
                    if mla:
                        P.op("pe", lambda e, sb_=sb_, kt=kt, qt=qt, kb=kb, qb=qb: e.matmul(
                            banks[sb_][:, :], kt[0:64, 1, kb * 128:(kb + 1) * 128], qt[0:64, 1, qb * 512:(qb + 1) * 512],
                            start=False, stop=True), reads=[kT, qT], writes=[bankT[sb_]])
                    pt, pT, _ = ptr.next()
                    P.op("act", lambda e, pt=pt, sb_=sb_, scale=scale: e.activation(pt[:], banks[sb_][:, :], AF.Exp, scale=scale),
                         reads=[bankT[sb_]], writes=[pT])
                    for sub in range(4):
                        a = qb * 4 + sub
                        bk, col = a // 2, (a % 2) * 256
                        first = (pc == 0 and kb == 0)
                        last = (pc == NPIECE - 1 and kb == KPB - 1)
                        P.op("pe", lambda e, bk=bk, col=col, pt=pt, sub=sub, vt=vt, kb=kb, first=first, last=last, a=a: e.matmul(
                            banks[bk][:, col:col + 129], pt[:, sub * 128:(sub + 1) * 128], vt[:, kb * 130:kb * 130 + 129],
                            start=(first and a % 2 == 0), stop=last, skip_group_check=True),
                            reads=[pT, vT_], writes=[bankT[bk]])
        for a in range(8):
            finalize(a // 2, (a % 2) * 256, ost, oT, a)
        P.dma("sp", o_v[:, :, hd * 128:(hd + 1) * 128], ost[:], osem, reads=[oT])

    knr = Ring(cx, "kn", [128, NHALO * 128], BF16, 2)
    vnr = Ring(cx, "vn", [128, NHALO * 130], BF16, 2)
    bnr = Ring(cx, "bn", [128, NBT * 128], F32, 2)
    snr = Ring(cx, "sn", [128, 768], F32, 2, dma=False)
    pnr = Ring(cx, "pn", [128, 768], BF16, 2, dma=False)
    scale = 128.0 ** -0.5
    ai = 0
    for h in range(6):
        qt, qT, qs = qr.next()
        P.dma("sp", qt[:, 0, :], qn[h], qs, writes=[qT])
        kt, kT, ks = knr.next()
        vt, vT_, vs = vnr.next()
        bt, bT, bs = bnr.next()
        P.dma("sp", kt[:], kn[h], ks, writes=[kT])
        P.dma("sp", vt[:], vn[h], vs, writes=[vT_])
        P.dma("pool", bt[:], bn[h], bs, writes=[bT])
        ost, oT, osem = osr.next()
        for p in range(NTC):
            slots = na_slots(p)
            ns = len(slots)
            sA, sB = 4 + (sbank[0] % 2) * 2, 5 + (sbank[0] % 2) * 2
            sbank[0] += 1
            for si, dl in enumerate(slots):
                sb_, c0 = (sA, si * 128) if si < 4 else (sB, (si - 4) * 128)
                hc = p + dl + 3
                P.op("pe", lambda e, sb_=sb_, c0=c0, kt=kt, qt=qt, hc=hc, p=p: e.matmul(
                    banks[sb_][:, c0:c0 + 128], kt[:, hc * 128:(hc + 1) * 128], qt[:, 0, p * 128:(p + 1) * 128],
                    start=True, stop=True, skip_group_check=True), reads=[kT, qT], writes=[bankT[sb_]])
            sn, sT, _ = snr.next()
            pn, pT, _ = pnr.next()
            for (sb_, s0, s1) in ((sA, 0, min(ns, 4)), (sB, 4, ns)):
                if s1 <= s0:
                    continue
                for si in range(s0, s1):
                    ti = na_tile_index(p, si)
                    c0 = (si - s0) * 128
                    P.op("dve", lambda e, sn=sn, si=si, sb_=sb_, c0=c0, bt=bt, ti=ti: e.scalar_tensor_tensor(
                        sn[:, si * 128:(si + 1) * 128], banks[sb_][:, c0:c0 + 128], scale, bt[:, ti * 128:(ti + 1) * 128],
                        ALU.mult, ALU.add), reads=[bankT[sb_], bT], writes=[sT])
            P.op("act", lambda e, pn=pn, sn=sn, ns=ns: e.activation(pn[:, 0:ns * 128], sn[:, 0:ns * 128], AF.Exp),
                 reads=[sT], writes=[pT])
            bk = ai % 4
            ai += 1
            for si, dl in enumerate(slots):
                hc = p + dl + 3
                P.op("pe", lambda e, bk=bk, pn=pn, si=si, vt=vt, hc=hc, ns=ns: e.matmul(
                    banks[bk][:, 0:129], pn[:, si * 128:(si + 1) * 128], vt[:, hc * 130:hc * 130 + 129],
                    start=(si == 0), stop=(si == ns - 1), skip_group_check=True), reads=[pT, vT_], writes=[bankT[bk]])
            finalize(bk, 0, ost, oT, p)
        P.dma("sp", o_v[:, :, (10 + h) * 128:(11 + h) * 128], ost[:], osem, reads=[oT])

    P.final_dsems = osr.ds
    return cx.finish()


def na_bias_tiles(rpb_l, core):
    H = 6
    out = np.full((H, NBT, 128, 128), NEG, np.float32)

    def tile(c, kc_):
        qi = np.arange(128)
        r = 2 * c + qi // 64
        cq = qi % 64
        ki = np.arange(128)
        kr = 2 * kc_ + ki // 64
        kcol = ki % 64
        rs = np.clip(r - 4, 0, 128 - 8)
        cs = np.clip(cq - 8, 0, 64 - 16)
        valid = ((kr[:, None] >= rs[None, :]) & (kr[:, None] < rs[None, :] + 8) &
                 (kcol[:, None] >= cs[None, :]) & (kcol[:, None] < cs[None, :] + 16))
        dr = kr[:, None] - r[None, :] + 7
        dc = np.clip(kcol[:, None] - cq[None, :] + 15, 0, 30)
        valid &= (dr >= 0) & (dr <= 14) & (kc_ >= 0) & (kc_ < 64)
        return valid, np.clip(dr, 0, 14), dc

    def fill(ti, c, kc_):
        valid, dr, dc = tile(c, kc_)
        for h in range(H):
            vals = rpb_l[h][dr, dc]
            out[h, ti] = np.where(valid, vals, np.float32(NEG))

    for si, dl in enumerate([-2, -1, 0, 1, 2]):
        fill(si, 10, 10 + dl)
    for p in (0, 1, 6, 7):
        c = core * 8 + p
        for si, dl in enumerate(na_slots(p)):
            fill(na_tile_index(p, si), c, c + dl)
    return np.ascontiguousarray(out.transpose(0, 2, 1, 3).reshape(H, 128, NBT * 128))


def attn_inputs(inp, l, qkv_all):
    q = qkv_all
    def cols(name, n):
        return q[:, OFF[name]:OFF[name] + n]
    qm = cols("qm", 1152).reshape(SEQ, 6, 192)
    km = cols("km", 1152).reshape(SEQ, 6, 192)
    vm = cols("vm", 768).reshape(SEQ, 6, 128)
    qg = cols("qg", 512).reshape(SEQ, 4, 128)
    kg = cols("kg", 256).reshape(SEQ, 2, 128)
    vg = cols("vg", 256).reshape(SEQ, 2, 128)
    qn_ = cols("qn", 768).reshape(SEQ, 6, 128)
    kn_ = cols("kn", 768).reshape(SEQ, 6, 128)
    vn_ = cols("vn", 768).reshape(SEQ, 6, 128)
    qa_all = np.concatenate([qm[:, :, 0:128], qg], axis=1).transpose(1, 2, 0)
    qb_all = qm[:, :, 128:192].transpose(1, 2, 0)
    ka_all = np.ascontiguousarray(np.concatenate([km[:, :, 0:128], kg[:, [0, 0, 1, 1], :]], axis=1).transpose(1, 2, 0))
    kb_all = np.ascontiguousarray(km[:, :, 128:192].transpose(1, 2, 0))
    v_all = np.concatenate([vm, vg[:, [0, 0, 1, 1], :]], axis=1)
    va = np.zeros((10, NPIECE, 128, KPB, 130), NPBF)
    va[..., 0:128] = v_all.reshape(NPIECE, KPB, 128, 10, 128).transpose(3, 0, 2, 1, 4)
    va[..., 128] = 1.0
    va = va.reshape(10, NPIECE, 128, KPB * 130)
    qn_all = qn_.transpose(1, 2, 0)
    maps = []
    for j in range(NCORES):
        sl = slice(j * TOK, (j + 1) * TOK)
        knh = np.zeros((6, 128, NHALO, 128), NPBF)
        vnh = np.zeros((6, 128, NHALO, 130), NPBF)
        for hc in range(NHALO):
            c = j * 8 + hc - 3
            if 0 <= c < 64:
                blk = slice(c * 128, (c + 1) * 128)
                knh[:, :, hc, :] = kn_[blk].transpose(1, 2, 0)
                vnh[:, :, hc, 0:128] = vn_[blk].transpose(1, 0, 2)
                vnh[:, :, hc, 128] = 1.0
        maps.append({
            "qa": np.ascontiguousarray(qa_all[:, :, sl]), "qb": np.ascontiguousarray(qb_all[:, :, sl]),
            "ka": ka_all, "kb": kb_all, "va": va,
            "qn": np.ascontiguousarray(qn_all[:, :, sl]),
            "kn": knh.reshape(6, 128, NHALO * 128), "vn": vnh.reshape(6, 128, NHALO * 130),
            "bn": na_bias_tiles(inp["na_rpb"][l], j),
        })
    return maps


CAP = 128
NR = 4
NSLOT = 4
BIG = 1.0e4


class Slot:
    def __init__(self, cx, arena, i):
        self.v = arena[:, i, :]
        self.Tw = T()
        self.Th = [T(), T()]
        self.sem = cx.P.dsem(f"s_ar{i}")
        self.hsem = [cx.P.dsem(f"s_ar{i}h0"), cx.P.dsem(f"s_ar{i}h1")]

    def wW(self):
        return [self.Tw, self.Th[0], self.Th[1]]

    def rW(self):
        return [self.Tw]

    def half(self, i):
        return self.v.bitcast(F32)[:, i * D:(i + 1) * D]

    def wH(self, i):
        return [self.Th[i], self.Tw]

    def rH(self, i):
        return [self.Th[i]]


def build_post():
    cx = Ctx()
    P = cx.P
    x_d = cx.din("x", [TOK, D], F32)
    o_d = cx.din("o", [TOK, D], F32)
    vT_d = cx.din("vT", [128, KD], F32)
    rows = cx.din("rows", [5, D], F32)
    w_out = cx.din("w_out", [D, D], F32)
    wr_d = cx.din("wr", [D, 36], F32)
    rb_d = cx.din("rb", [1, 36], F32)
    wg_d = cx.din("wg", [NE, D, DE], F32)
    wu_d = cx.din("wu", [NE, D, DE], F32)
    wd_d = cx.din("wd", [NE, DE, D], F32)
    xo = cx.dout("xo", [TOK, D], F32)
    cx.psum_banks()
    cx.ident()
    banks, bankT = cx.banks, cx.bankT
    st = Stat(cx, "stat", 128)
    s0 = P.dsem("s0")

    vT = cx.sb("vT", [128, KD], F32)
    tv = T()
    P.dma("sp", vT[:], vT_d, s0, writes=[tv])
    mhalf = cx.sb("mhalf", [128, 8], F32)
    tmh = T()
    P.op("pool", lambda e: e.memset(mhalf[:], -0.5), writes=[tmh])

    def rstd_of(ss, tss, w, div):
        ms, tms = st.get(w)
        rs, trs = st.get(w)
        P.op("pool", lambda e: e.tensor_scalar(ms, ss, 1.0 / div, EPS, ALU.mult, ALU.add), reads=[tss], writes=[tms])
        P.op("pool", lambda e: e.tensor_tensor(rs, ms, mhalf[:, 0:w], ALU.pow), reads=[tms, tmh], writes=[trs])
        return rs, trs

    xs = cx.sb("xs", [128, NTC, D], F32)
    tx = [T() for _ in range(NTC)]
    sx = P.dsem("sx")
    for tc in range(NTC):
        P.dma("sp", xs[:, tc, :], x_d[tc * 128:(tc + 1) * 128, :], sx, writes=[tx[tc]])
    gbc = cx.sb("gbc", [128, D], F32)
    tgbc = T()
    sg = P.dsem("sg")
    P.dma("sp", gbc[:], rows[0:1, :].to_broadcast([128, D]), sg, writes=[tgbc])

    arena = cx.sb("arena", [128, NSLOT, 8192], BF16)
    SL = [Slot(cx, arena, i) for i in range(NSLOT)]
    hb = cx.sb("hb", [128, NTC, D], BF16)
    thb = [T() for _ in range(NTC)]
    xer = Ring(cx, "xe", [128, KD, CAP], BF16, 2, dma=False)
    junk = xer.bufs[0][:].rearrange("p k s -> p (k s)")
    tjunk = xer.ts[0]

    GRP = ((0, 768), (768, 1280), (1280, 2048))
    for tc in range(NTC):
        hi = tc % 2
        ot = SL[2].half(hi)
        P.dma("sp", ot, o_d[tc * 128:(tc + 1) * 128, :], SL[2].hsem[hi], writes=SL[2].wH(hi))
        ss, tss = st.get(3)
        for gi, (a, b) in enumerate(GRP):
            P.op("act", lambda e, ot=ot, a=a, b=b, ss=ss, gi=gi: e.activation(junk[:, a:b], ot[:, a:b], AF.Square, accum_out=ss[:, gi:gi + 1]),
                 reads=SL[2].rH(hi), writes=[tjunk, tss])
        ms, tms = st.get(3)
        for gi, (a, b) in enumerate(GRP):
            P.op("pool", lambda e, ms=ms, ss=ss, gi=gi, a=a, b=b: e.tensor_scalar(ms[:, gi:gi + 1], ss[:, gi:gi + 1], 1.0 / (b - a), EPS, ALU.mult, ALU.add),
                 reads=[tss], writes=[tms])
        rs, trs = st.get(3)
        P.op("pool", lambda e, rs=rs, ms=ms: e.tensor_tensor(rs, ms, mhalf[:, 0:3], ALU.pow), reads=[tms, tmh], writes=[trs])
        for gi, (a, b) in enumerate(GRP):
            P.op("dve", lambda e, ot=ot, a=a, b=b, rs=rs, gi=gi, tc=tc: e.tensor_scalar(hb[:, tc, a:b], ot[:, a:b], rs[:, gi:gi + 1], None, ALU.mult),
                 reads=SL[2].rH(hi) + [trs], writes=[thb[tc]])
    oTv = arena[:, 0:2, :].rearrange("p s n -> p (s n)").rearrange("p (k t) -> p k t", k=KD)
    for k in range(KD):
        bk = 6 + (k % 2)
        psv = banks[bk][:].bitcast(BF16)
        for tc in range(NTC):
            P.op("pe", lambda e, psv=psv, tc=tc, k=k: e.transpose(psv[:, tc * 128:(tc + 1) * 128], hb[:, tc, k * 128:(k + 1) * 128], cx.idb[:]),
                 reads=[thb[tc], cx.tid], writes=[bankT[bk]])
        P.op("act", lambda e, psv=psv, k=k: e.activation(oTv[:, k, :], psv, AF.Identity, scale=vT[:, k:k + 1]),
             reads=[bankT[bk], tv], writes=SL[k // 8].wW())

    scr = Ring(cx, "scr", [128, 512], F32, 2, dma=False)
    it = 0
    wv = SL[2].v.rearrange("p (k n) -> p k n", k=KD)
    for cb in range(4):
        for kk in range(0, KD, 4):
            P.dma("pool", wv[:, kk:kk + 4, :], w_out[kk * 128:(kk + 4) * 128, cb * 512:(cb + 1) * 512].rearrange("(k p) n -> p k n", p=128),
                  SL[2].sem, writes=SL[2].wW())
        for tc in range(NTC):
            bk = it % 4
            it += 1
            for k in range(KD):
                P.op("pe", lambda e, bk=bk, k=k, tc=tc: e.matmul(banks[bk][:, :], oTv[:, k, tc * 128:(tc + 1) * 128], wv[:, k, :],
                                                                 start=(k == 0), stop=(k == KD - 1)),
                     reads=SL[0].rW() + SL[1].rW() + SL[2].rW(), writes=[bankT[bk]])
            tm, tmT, _ = scr.next()
            P.op("dve", lambda e, tm=tm, bk=bk, cb=cb: e.tensor_tensor(tm[:], banks[bk][:, :], gbc[:, cb * 512:(cb + 1) * 512], ALU.mult),
                 reads=[bankT[bk], tgbc], writes=[tmT])
            P.op("pool", lambda e, tm=tm, tc=tc, cb=cb: e.tensor_tensor(xs[:, tc, cb * 512:(cb + 1) * 512], xs[:, tc, cb * 512:(cb + 1) * 512], tm[:], ALU.add),
                 reads=[tmT, tx[tc]], writes=[tx[tc]])

    A2b = SL[2].v.bitcast(F32)
    P.dma("sp", A2b[:, 0:D], rows[1:2, :].to_broadcast([128, D]), SL[2].sem, writes=SL[2].wW())
    P.dma("sp", A2b[:, D:2 * D], rows[3:4, :].to_broadcast([128, D]), SL[2].sem, writes=SL[2].wW())
    scb = SL[0].half(1)
    P.dma("sp", scb, rows[2:3, :].to_broadcast([128, D]), SL[0].hsem[1], writes=SL[0].wH(1))
    P.op("dve", lambda e: e.scalar_tensor_tensor(A2b[:, 0:D], scb, 1.0, A2b[:, 0:D], ALU.add, ALU.mult),
         reads=SL[0].rH(1) + SL[2].rW(), writes=SL[2].wW())
    wr = cx.sb("wr", [128, KD, 36], F32)
    rbb = cx.sb("rbb", [128, 36], F32)
    twr, trb = T(), T()
    P.dma("sp", wr[:], wr_d.rearrange("(k p) n -> p k n", p=128), s0, writes=[twr])
    P.dma("sp", rbb[:], rb_d.to_broadcast([128, 36]), s0, writes=[trb])
    ybig = cx.sb("ybig", [128, 2, D], BF16)
    h2T = ybig[:].rearrange("p a d -> p (a d)").bitcast(F32).rearrange("p (k t) -> p k t", k=KD)
    th2T = T()
    ybT = [T(), T()]
    A_f = cx.sb("A_f", [128, NTC, 32], F32)
    A_b = cx.sb("A_b", [128, NTC, 32], BF16)
    Wt_f = cx.sb("Wt_f", [128, NTC, 32], F32)
    rank_f = cx.sb("rank_f", [128, NTC, 32], F32)
    tA = [T() for _ in range(NTC)]
    tW = [T() for _ in range(NTC)]
    tR = [T() for _ in range(NTC)]
    rt = cx.sb("rt", [128, 256], F32)
    tr = T()
    for tc in range(NTC):
        ss, tss = st.get(1)
        P.op("act", lambda e, tc=tc, ss=ss: e.activation(junk[:], xs[:, tc, :], AF.Square, accum_out=ss),
             reads=[tx[tc]], writes=[tjunk, tss])
        rs, trs = rstd_of(ss, tss, 1, D)
        hi = tc % 2
        hf = SL[0].half(hi)
        P.op("dve", lambda e, hf=hf, tc=tc, rs=rs: e.scalar_tensor_tensor(hf, xs[:, tc, :], rs, A2b[:, 0:D], ALU.mult, ALU.mult),
             reads=[tx[tc], trs] + SL[2].rW(), writes=SL[0].wH(hi))
        P.op("pool", lambda e, hf=hf: e.tensor_tensor(hf, hf, A2b[:, D:2 * D], ALU.add), reads=SL[0].rH(hi) + SL[2].rW(), writes=SL[0].wH(hi))
        P.op("act", lambda e, hf=hf, tc=tc: e.copy(hb[:, tc, :], hf), reads=SL[0].rH(hi), writes=[thb[tc]])
        for k4 in range(4):
            bk = 4 + (k4 % 2)
            for kk in range(4):
                k = k4 * 4 + kk
                P.op("pe", lambda e, bk=bk, kk=kk, k=k, hf=hf: e.transpose(banks[bk][:, kk * 128:(kk + 1) * 128], hf[:, k * 128:(k + 1) * 128], cx.idf[:]),
                     reads=SL[0].rH(hi) + [cx.tid], writes=[bankT[bk]])
            P.op("act", lambda e, bk=bk, k4=k4: e.copy(h2T[:, k4 * 4:(k4 + 1) * 4, :], banks[bk][:, :].rearrange("p (k t) -> p k t", k=4)),
                 reads=[bankT[bk]], writes=[th2T])
        for k in range(KD):
            P.op("pe", lambda e, k=k: e.matmul(banks[6][:, 0:36], h2T[:, k, :], wr[:, k, :], start=(k == 0), stop=(k == KD - 1)),
                 reads=[th2T, twr], writes=[bankT[6]])
        r = rt
        lg, gmax, ngmax, gsel, gexp, gsum, pg = r[:, 0:36], r[:, 36:37], r[:, 37:38], r[:, 38:42], r[:, 42:46], r[:, 46:47], r[:, 47:48]
        pen, em, m1, oh1 = r[:, 48:52], r[:, 52:84], r[:, 84:85], r[:, 88:120]
        em2, m2, oh2 = r[:, 120:152], r[:, 152:153], r[:, 160:192]
        nm1, w2, den, rden, wt1, wt2 = r[:, 192:193], r[:, 193:194], r[:, 194:195], r[:, 195:196], r[:, 196:197], r[:, 197:198]
        dv = lambda fn, rd=(), wr_=(): P.op("dve", fn, reads=[tr] + list(rd), writes=[tr] + list(wr_))
        dv(lambda e: e.tensor_tensor(lg, banks[6][:, 0:36], rbb[:], ALU.add), rd=[bankT[6], trb])
        dv(lambda e: e.tensor_reduce(gmax, lg[:, 0:4], AX.X, ALU.max))
        dv(lambda e: e.tensor_scalar(ngmax, gmax, -1.0, None, ALU.mult))
        dv(lambda e: e.tensor_scalar(gsel, lg[:, 0:4], gmax, None, ALU.is_ge))
        P.op("act", lambda e: e.activation(gexp, lg[:, 0:4], AF.Exp, bias=ngmax, accum_out=gsum), reads=[tr], writes=[tr])
        dv(lambda e: e.reciprocal(pg, gsum))
        dv(lambda e: e.tensor_scalar(pen, gsel, BIG, -BIG, ALU.mult, ALU.add))
        dv(lambda e: e.tensor_tensor(em.rearrange("p (g j) -> p g j", g=4), lg[:, 4:36].rearrange("p (g j) -> p g j", g=4),
                                     bc(pen.unsqueeze(2), [128, 4, 8]), ALU.add))
        dv(lambda e: e.tensor_reduce(m1, em, AX.X, ALU.max))
        dv(lambda e: e.tensor_scalar(oh1, em, m1, None, ALU.is_ge))
        dv(lambda e: e.scalar_tensor_tensor(em2, oh1, -BIG, em, ALU.mult, ALU.add))
        dv(lambda e: e.tensor_reduce(m2, em2, AX.X, ALU.max))
        dv(lambda e: e.tensor_scalar(oh2, em2, m2, None, ALU.is_ge))
        dv(lambda e: e.tensor_scalar(nm1, m1, -1.0, None, ALU.mult))
        P.op("act", lambda e: e.activation(w2, m2, AF.Exp, bias=nm1), reads=[tr], writes=[tr])
        dv(lambda e: e.tensor_scalar(den, w2, 1.0, None, ALU.add))
        dv(lambda e: e.reciprocal(rden, den))
        dv(lambda e: e.tensor_tensor(wt1, pg, rden, ALU.mult))
        dv(lambda e: e.tensor_tensor(wt2, pg, wt1, ALU.subtract))
        dv(lambda e, tc=tc: e.tensor_tensor(A_f[:, tc, :], oh1, oh2, ALU.add), wr_=[tA[tc]])
        dv(lambda e, tc=tc: e.tensor_copy(A_b[:, tc, :], A_f[:, tc, :]), rd=[tA[tc]], wr_=[tA[tc]])
        dv(lambda e, tc=tc: e.tensor_scalar(Wt_f[:, tc, :], oh1, wt1, None, ALU.mult), wr_=[tW[tc]])
        dv(lambda e, tc=tc: e.scalar_tensor_tensor(Wt_f[:, tc, :], oh2, wt2, Wt_f[:, tc, :], ALU.mult, ALU.add), rd=[tW[tc]], wr_=[tW[tc]])

    onesb = cx.sb("onesb", [128, 128], BF16)
    Lb = cx.sb("Lb", [128, 128], BF16)
    iof = cx.sb("iof", [128, CAP], F32)
    ioi = cx.sb("ioi", [128, CAP], I32)
    flags = cx.sb("flags", [1, NR * 32], I32)
    tcst, tflag = T(), T()
    P.op("pool", lambda e: e.memset(onesb[:], 1.0), writes=[tcst])
    P.op("pool", lambda e: e.iota(ioi[:], pattern=[[1, CAP]], base=0, channel_multiplier=-1), writes=[tcst])
    P.op("dve", lambda e: e.tensor_single_scalar(Lb[:], ioi[:], 0, ALU.is_gt), reads=[tcst], writes=[tcst])
    P.op("pool", lambda e: e.iota(ioi[:], pattern=[[1, CAP]], base=0, channel_multiplier=0), reads=[tcst], writes=[tcst])
    P.op("dve", lambda e: e.tensor_copy(iof[:], ioi[:]), reads=[tcst], writes=[tcst])
    for tc in range(NTC):
        for c2 in range(tc + 1):
            P.op("pe", lambda e, c2=c2, tc=tc: e.matmul(banks[7][:, 0:32], (Lb if c2 == tc else onesb)[:], A_b[:, c2, :],
                                                         start=(c2 == 0), stop=(c2 == tc)),
                 reads=[tA[c2], tcst], writes=[bankT[7]])
        P.op("dve", lambda e, tc=tc: e.tensor_tensor(rank_f[:, tc, :], banks[7][:, 0:32], A_f[:, tc, :], ALU.mult),
             reads=[bankT[7], tA[tc]], writes=[tR[tc]])
        P.op("dve", lambda e, tc=tc: e.scalar_tensor_tensor(rank_f[:, tc, :], A_f[:, tc, :], -1.0, rank_f[:, tc, :], ALU.add, ALU.add),
             reads=[tR[tc], tA[tc]], writes=[tR[tc]])
    for c2 in range(NTC):
        P.op("pe", lambda e, c2=c2: e.matmul(banks[7][0:1, 64:96], onesb[:, 0:1], A_b[:, c2, :], start=(c2 == 0), stop=(c2 == NTC - 1)),
             reads=[tA[c2], tcst], writes=[bankT[7]])
    for r in range(NR):
        P.op("dve", lambda e, r=r: e.tensor_single_scalar(flags[0:1, r * 32:(r + 1) * 32], banks[7][0:1, 64:96], r * CAP - 0.5, ALU.is_gt),
             reads=[bankT[7]], writes=[tflag])

    P.dma("sp", gbc[:], rows[4:5, :].to_broadcast([128, D]), sg, writes=[tgbc])

    selr = Ring(cx, "sel", [128, NTC, CAP], BF16, 2, dma=False)
    selTr = Ring(cx, "selT", [128, NTC * CAP], BF16, 2, dma=False)
    actr = Ring(cx, "actT", [128, 4, CAP], BF16, 1, dma=False)
    wslot = [1]

    def wload(src3, n_k):
        sl = SL[wslot[0] % NSLOT]
        wslot[0] += 1
        wv_ = sl.v.rearrange("p (k n) -> p k n", k=n_k)
        step = max(1, n_k // 4)
        for kk in range(0, n_k, step):
            P.dma("pool", wv_[:, kk:kk + step, :], src3[kk * 128:(kk + step) * 128, :].rearrange("(k p) n -> p k n", p=128),
                  sl.sem, writes=sl.wW())
        return wv_, sl

    st_ = {}
    W = {}
    ycnt = [0]

    def LW_gu(e):
        W[e] = dict(g=wload(wg_d[e], KD), u=wload(wu_d[e], KD))

    def LW_d(e):
        W[e]["d"] = wload(wd_d[e], 4)

    def G(e, r):
        sel, selT_, _ = selr.next()
        for c in range(NTC):
            P.op("dve", lambda en, sel=sel, c=c, e=e, r=r: en.tensor_scalar(sel[:, c, :], iof[:], rank_f[:, c, e:e + 1], -float(r * CAP),
                                                                             ALU.subtract, ALU.is_equal),
                 reads=[tR[c], tcst], writes=[selT_])
        xe, xeT, _ = xer.next()
        for k4 in range(4):
            bk = k4
            for kk in range(4):
                k = k4 * 4 + kk
                for c in range(NTC):
                    P.op("pe", lambda en, bk=bk, kk=kk, k=k, c=c, sel=sel: en.matmul(
                        banks[bk][:, kk * 128:(kk + 1) * 128], hb[:, c, k * 128:(k + 1) * 128], sel[:, c, :],
                        start=(c == 0), stop=(c == NTC - 1), skip_group_check=True),
                        reads=[thb[c], selT_], writes=[bankT[bk]])
            P.op("act", lambda en, bk=bk, k4=k4, xe=xe: en.copy(xe[:, k4 * 4:(k4 + 1) * 4, :], banks[bk][:, :].rearrange("p (k s) -> p k s", k=4)),
                 reads=[bankT[bk]], writes=[xeT])
        sT_, sTT, _ = selTr.next()
        psv = banks[6][:].bitcast(BF16)
        for c in range(NTC):
            P.op("pe", lambda en, c=c, sel=sel, psv=psv: en.transpose(psv[:, c * 128:(c + 1) * 128], sel[:, c, :], cx.idb[:]),
                 reads=[selT_, cx.tid], writes=[bankT[6]])
        P.op("act", lambda en, sT_=sT_, psv=psv: en.copy(sT_[:], psv), reads=[bankT[6]], writes=[sTT])
        st_[(e, r)] = dict(xe=xe, xeT=xeT, sT=sT_, sTT=sTT)

    def C(e, r):
        s = st_[(e, r)]
        for (key, bk) in (("g", 4), ("u", 5)):
            w, wS = W[e][key]
            for f in range(4):
                for k in range(KD):
                    P.op("pe", lambda en, w=w, bk=bk, f=f, k=k, xe=s["xe"]: en.matmul(
                        banks[bk][:, f * 128:(f + 1) * 128], w[:, k, f * 128:(f + 1) * 128], xe[:, k, :],
                        start=(k == 0), stop=(k == KD - 1), skip_group_check=True),
                        reads=wS.rW() + [s["xeT"]], writes=[bankT[bk]])
        sgl, sglT, _ = scr.next()
        act, actT_, _ = actr.next()
        P.op("act", lambda en, sgl=sgl: en.activation(sgl[:], banks[4][:, :], AF.Silu), reads=[bankT[4]], writes=[sglT])
        P.op("dve", lambda en, act=act, sgl=sgl: en.tensor_tensor(act[:].rearrange("p f s -> p (f s)"), sgl[:], banks[5][:, :], ALU.mult),
             reads=[sglT, bankT[5]], writes=[actT_])
        s["act"], s["actT"] = act, actT_

    def Dn(e, r):
        s = st_[(e, r)]
        wd, wdS = W[e]["d"]
        yi = ycnt[0] % 2
        ycnt[0] += 1
        yb = ybig[:, yi, :]
        for cb in range(4):
            for f in range(4):
                P.op("pe", lambda en, cb=cb, f=f, wd=wd, act=s["act"]: en.matmul(
                    banks[cb][:, :], act[:, f, :], wd[:, f, cb * 512:(cb + 1) * 512], start=(f == 0), stop=(f == 3)),
                    reads=[s["actT"]] + wdS.rW(), writes=[bankT[cb]])
            P.op("dve", lambda en, cb=cb, yb=yb: en.tensor_tensor(yb[:, cb * 512:(cb + 1) * 512], banks[cb][:, :], gbc[:, cb * 512:(cb + 1) * 512], ALU.mult),
                 reads=[bankT[cb], tgbc], writes=[ybT[yi], th2T])
        s["yb"], s["ybT"] = yb, ybT[yi]

    sc_i = [0]

    def S(e, r):
        s = st_[(e, r)]
        for c in range(NTC):
            for cb in range(4):
                bk = 6 + (sc_i[0] % 2)
                sc_i[0] += 1
                P.op("pe", lambda en, bk=bk, c=c, cb=cb, s=s: en.matmul(
                    banks[bk][:, :], s["sT"][:, c * 128:(c + 1) * 128], s["yb"][:, cb * 512:(cb + 1) * 512], start=True, stop=True),
                    reads=[s["sTT"], s["ybT"]], writes=[bankT[bk]])
                P.op("dve", lambda en, bk=bk, c=c, cb=cb, e=e: en.scalar_tensor_tensor(
                    xs[:, c, cb * 512:(cb + 1) * 512], banks[bk][:, :], Wt_f[:, c, e:e + 1], xs[:, c, cb * 512:(cb + 1) * 512],
                    ALU.mult, ALU.add), reads=[bankT[bk], tW[c], tx[c]], writes=[tx[c]])
        del st_[(e, r)]

    LW_gu(0)
    G(0, 0)
    C(0, 0)
    G(1, 0)
    for e in range(NE):
        LW_d(e)
        Dn(e, 0)
        S(e, 0)
        for r in range(1, NR):
            P.cond_begin(flags[0:1, r * 32 + e:r * 32 + e + 1], tflag)
            G(e, r)
            C(e, r)
            Dn(e, r)
            S(e, r)
            P.cond_end()
            xer.skip()
            selTr.skip()
        if e + 1 < NE:
            LW_gu(e + 1)
            C(e + 1, 0)
        if e + 2 < NE:
            G(e + 2, 0)

    so = P.dsem("so")
    for tc in range(NTC):
        P.dma("sp", xo[tc * 128:(tc + 1) * 128, :], xs[:, tc, :], so, reads=[tx[tc]])
    P.final_dsems = [so]
    return cx.finish()


def post_inputs(inp, l, x_full, o_full, mod):
    m = mod[l].reshape(6, D)
    vT = colT(inp["mix_out_norm_g"][l])
    rows = np.ascontiguousarray(np.stack([m[2], inp["norm2_g"][l], m[4], m[3], m[5]]).astype(np.float32))
    wr = np.ascontiguousarray(np.concatenate([inp["router_group_w"][l], inp["router_expert_w"][l]], axis=1))
    rb = np.concatenate([inp["router_group_b"][l], inp["router_expert_b"][l]]).reshape(1, 36).astype(np.float32)
    maps = []
    for j in range(NCORES):
        sl = slice(j * TOK, (j + 1) * TOK)
        maps.append({"x": np.ascontiguousarray(x_full[sl]), "o": np.ascontiguousarray(o_full[sl]), "vT": vT, "rows": rows,
                     "w_out": inp["w_out"][l], "wr": wr, "rb": rb,
                     "wg": inp["expert_w_gate"][l], "wu": inp["expert_w_up"][l], "wd": inp["expert_w_down"][l]})
    return maps


_PROGS = {}


def _prog(name, builder):
    if name not in _PROGS:
        _PROGS[name] = builder()
    return _PROGS[name]


def _run(nc, maps):
    res = run_bass_kernel_spmd(nc, maps, core_ids=list(range(NCORES)))
    return res.results


def kernel(**inp):
    inp = {k: np.asarray(v) for k, v in inp.items()}
    x = np.ascontiguousarray(inp["x"][0], dtype=np.float32)
    c = inp["c"].astype(np.float32)
    tabs = rope_tables()
    cT = np.ascontiguousarray(c.reshape(KD, 128).T)
    maps = []
    for j in range(NCORES):
        sl = slice(j * MODC, (j + 1) * MODC)
        maps.append({"cT": cT, "w": np.ascontiguousarray(inp["ada_w"][:, :, sl]),
                     "b": np.ascontiguousarray(inp["ada_b"][:, sl]).reshape(1, -1)})
    r = _run(_prog("ada", build_ada), maps)
    mod = np.concatenate([np.asarray(q["o"]).reshape(DEPTH, MODC) for q in r], axis=1)
    for l in range(DEPTH):
        r = _run(_prog("pre", build_pre), pre_inputs(inp, l, x, mod, tabs))
        qkv_all = np.concatenate([np.asarray(q["qkv"]) for q in r], axis=0)
        if qkv_all.dtype != NPBF:
            qkv_all = qkv_all.view(NPBF) if qkv_all.dtype.itemsize == 2 else qkv_all.astype(NPBF)
        r = _run(_prog("attn", build_attn), attn_inputs(inp, l, qkv_all))
        o_all = np.concatenate([np.asarray(q["o"]) for q in r], axis=0)
        r = _run(_prog("post", build_post), post_inputs(inp, l, x, o_all, mod))
        x = np.concatenate([np.asarray(q["xo"]) for q in r], axis=0)
    return np.ascontiguousarray(x[None].astype(np.float32))
```

```python
import numpy as np
from contextlib import ExitStack
import ml_dtypes
import concourse.bass as bass
import concourse.mybir as mybir
from concourse.bass_utils import run_bass_kernel_spmd

F32 = mybir.dt.float32
BF16 = mybir.dt.bfloat16
I32 = mybir.dt.int32
U32 = mybir.dt.uint32
AF = mybir.ActivationFunctionType
ALU = mybir.AluOpType
AX = mybir.AxisListType
NPBF = ml_dtypes.bfloat16

NCORES = 8
D = 2048
SEQ = 8192
TOK = SEQ // NCORES
NTC = TOK // 128
DEPTH = 4
KD = D // 128
EPS = 1e-6
D_IN = 4032
NG, EPG, NE, DE = 4, 8, 32, 512

EPOCH = 20000


class T:
    __slots__ = ("name", "w", "r")

    def __init__(self, name=""):
        self.name = name
        self.w = None
        self.r = []


class DmaSem:
    def __init__(self, prog, name):
        self.sem = prog.new_sem(name)
        self.count = 0


class Op:
    __slots__ = ("eng", "fn", "deps", "need_inc", "semref", "is_dma", "idx", "kind", "flag")

    def __init__(self, eng, fn, is_dma=False, kind="op", flag=None):
        self.eng = eng
        self.fn = fn
        self.deps = []
        self.need_inc = False
        self.semref = None
        self.is_dma = is_dma
        self.kind = kind
        self.flag = flag


class Prog:
    ENGS = ("pe", "act", "dve", "pool", "sp")

    def __init__(self, nc, stack):
        self.nc = nc
        self.ops = {e: [] for e in self.ENGS}
        self.n = 0
        self._stack = stack
        self.nsem = 0
        self.final_dsems = []

    def new_sem(self, name):
        self.nsem += 1
        return self._stack.enter_context(self.nc.semaphore(f"{name}_{self.nsem}"))

    def dsem(self, name):
        return DmaSem(self, name)

    def _record(self, op, reads, writes):
        deps = []
        for t in reads:
            if t.w is not None:
                deps.append(t.w)
        for t in writes:
            if t.w is not None:
                deps.append(t.w)
            deps.extend(t.r)
        seen = set()
        for d in deps:
            if id(d) in seen or d is op:
                continue
            seen.add(id(d))
            if d.eng == "pe" and op.eng == "pe" and not d.is_dma and not op.is_dma:
                continue
            op.deps.append(d)
            d.need_inc = True
        for t in reads:
            t.r.append(op)
        for t in writes:
            t.w = op
            t.r = []
        op.idx = self.n
        self.n += 1
        self.ops[op.eng].append(op)
        return op

    def op(self, eng, fn, reads=(), writes=()):
        return self._record(Op(eng, fn), reads, writes)

    def cond_begin(self, flag_ap, tflag):
        for e in ("pe", "act", "dve", "pool"):
            self._record(Op(e, None, kind="cb", flag=flag_ap), [tflag], [])

    def cond_end(self):
        for e in ("pe", "act", "dve", "pool"):
            self._record(Op(e, None, kind="ce"), [], [])

    def dma(self, eng, out, in_, dsem, reads=(), writes=(), **kw):
        def fn(e, out=out, in_=in_, kw=kw):
            return e.dma_start(out=out, in_=in_, **kw)
        op = Op(eng, fn, is_dma=True)
        dsem.count += 16
        op.semref = (dsem.sem, dsem.count)
        return self._record(op, reads, writes)

    def emit(self, block):
        for e in self.ENGS:
            cnt = 0
            sem = None
            for op in self.ops[e]:
                if op.is_dma or op.kind != "op":
                    continue
                if op.need_inc:
                    if sem is None or cnt >= EPOCH:
                        sem = self.new_sem(f"p_{e}")
                        cnt = 0
                    cnt += 1
                    op.semref = (sem, cnt)

        def run(e_name, eng):
            waited = {}
            region = None
            creg = [None]
            ops = self.ops[e_name]
            skip_ce = False
            for oi, op in enumerate(ops):
                if op.kind == "ce":
                    if skip_ce:
                        skip_ce = False
                        continue
                    region["guard"].__exit__(None, None, None)
                    if region["incs"]:
                        eg = eng.Else()
                        eg.__enter__()
                        for (sm, n) in region["incs"].values():
                            eng.drain().then_inc(sm, n)
                        eg.__exit__(None, None, None)
                    waited = region["waited"]
                    region = None
                    continue
                if op.kind == "cb" and ops[oi + 1].kind == "ce":
                    skip_ce = True
                    continue
                need = {}
                for d in op.deps:
                    s, v = d.semref
                    k = id(s)
                    if waited.get(k, 0) >= v:
                        continue
                    if k not in need or need[k][1] < v:
                        need[k] = (s, v)
                for k, (s, v) in need.items():
                    eng.wait_ge(s, v)
                    waited[k] = v
                if op.kind == "cb":
                    if creg[0] is None:
                        creg[0] = eng.alloc_register(f"creg_{e_name}")
                    eng.load(creg[0], op.flag)
                    g = eng.If_ne(creg[0], 0)
                    g.__enter__()
                    region = dict(guard=g, waited=dict(waited), incs={})
                    continue
                ins = op.fn(eng)
                if op.is_dma:
                    assert region is None
                    ins.then_inc(op.semref[0], 16)
                elif op.need_inc:
                    ins.then_inc(op.semref[0], 1)
                    if region is not None:
                        k = id(op.semref[0])
                        region["incs"][k] = (op.semref[0], region["incs"].get(k, (None, 0))[1] + 1)

        @block.tensor
        def _(eng):
            run("pe", eng)

        @block.scalar
        def _(eng):
            run("act", eng)

        @block.vector
        def _(eng):
            run("dve", eng)

        @block.gpsimd
        def _(eng):
            run("pool", eng)

        @block.sync
        def _(eng):
            run("sp", eng)
            for ds in self.final_dsems:
                eng.wait_ge(ds.sem, ds.count)


class Ctx:
    def __init__(self):
        self.nc = bass.Bass("TRN2", target_bir_lowering=False)
        self.st = ExitStack()
        self.P = Prog(self.nc, self.st)
        self.banks = None
        self.n = 0

    def sb(self, name, shape, dt):
        return self.st.enter_context(self.nc.sbuf_tensor("sb_" + name, shape, dt))

    def din(self, name, shape, dt):
        return self.nc.dram_tensor(name, list(shape), dt, kind="ExternalInput").ap()

    def dout(self, name, shape, dt):
        return self.nc.dram_tensor(name, list(shape), dt, kind="ExternalOutput").ap()

    def psum_banks(self):
        self.banks = [self.st.enter_context(self.nc.psum_tensor(f"bank{i}", [128, 512], F32)) for i in range(8)]
        self.bankT = [T(f"bank{i}") for i in range(8)]

    def ident(self):
        P = self.P
        io = self.sb("ident_io", [128, 128], I32)
        idb = self.sb("ident_bf", [128, 128], BF16)
        idf = self.sb("ident_f", [128, 128], F32)
        tio, tid = T("io"), T("ident")
        P.op("pool", lambda e: e.iota(io[:], pattern=[[1, 128]], base=0, channel_multiplier=-1), writes=[tio])
        P.op("dve", lambda e: e.tensor_single_scalar(idb[:], io[:], 0, ALU.is_equal), reads=[tio], writes=[tid])
        P.op("dve", lambda e: e.tensor_single_scalar(idf[:], io[:], 0, ALU.is_equal), reads=[tio], writes=[tid])
        self.idb, self.idf, self.tid = idb, idf, tid

    def finish(self):
        block = self.st.enter_context(self.nc.Block())
        self.P.emit(block)
        self.st.close()
        return self.nc


class Ring:
    def __init__(self, cx, name, shape, dt, n, dma=True):
        self.bufs = [cx.sb(f"{name}{i}", shape, dt) for i in range(n)]
        self.ts = [T(f"{name}{i}") for i in range(n)]
        self.ds = [cx.P.dsem(f"s_{name}{i}") if dma else None for i in range(n)]
        self.i = 0
        self.n = n

    def next(self):
        i = self.i % self.n
        self.i += 1
        return self.bufs[i], self.ts[i], self.ds[i]

    def skip(self):
        self.i += 1


MODC = 6 * D // NCORES


def build_ada():
    cx = Ctx()
    P = cx.P
    cT = cx.din("cT", [128, KD], F32)
    w = cx.din("w", [DEPTH, D, MODC], F32)
    b = cx.din("b", [1, DEPTH * MODC], F32)
    o = cx.dout("o", [1, DEPTH * MODC], F32)
    cx.psum_banks()
    c_sb = cx.sb("c_sb", [128, KD], F32)
    cond = cx.sb("cond", [128, KD], F32)
    b_sb = cx.sb("b_sb", [1, DEPTH * MODC], F32)
    o_sb = cx.sb("o_sb", [1, DEPTH * MODC], F32)
    tc_, tcond, tb, to = T(), T(), T(), T()
    s0 = P.dsem("s0")
    so = P.dsem("so")
    P.dma("sp", c_sb[:], cT, s0, writes=[tc_])
    P.dma("sp", b_sb[:], b, s0, writes=[tb])
    P.op("act", lambda e: e.activation(cond[:], c_sb[:], AF.Silu), reads=[tc_], writes=[tcond])
    wr = Ring(cx, "w", [128, KD, 512], F32, 3)
    i = 0
    for l in range(DEPTH):
        for cb in range(MODC // 512):
            wt, wT, ws = wr.next()
            P.dma("sp" if i % 2 == 0 else "act", wt[:], w[l, :, cb * 512:(cb + 1) * 512].rearrange("(k p) n -> p k n", p=128),
                  ws, writes=[wT])
            bk = i % 2
            for k in range(KD):
                P.op("pe", lambda e, k=k, wt=wt, bk=bk: e.matmul(cx.banks[bk][0:1, :], cond[:, k:k + 1], wt[:, k, :],
                                                                  start=(k == 0), stop=(k == KD - 1)),
                     reads=[tcond, wT], writes=[cx.bankT[bk]])
            c0 = l * MODC + cb * 512
            P.op("dve", lambda e, bk=bk, c0=c0: e.tensor_tensor(o_sb[0:1, c0:c0 + 512], cx.banks[bk][0:1, :],
                                                                  b_sb[0:1, c0:c0 + 512], ALU.add),
                 reads=[cx.bankT[bk], tb], writes=[to])
            i += 1
    P.dma("sp", o, o_sb[:], so, reads=[to])
    P.final_dsems = [so]
    return cx.finish()


QKV_COLS = 6400
OFF = dict(qm=0, km=1152, vm=2304, qg=3072, kg=3584, vg=3840, qn=4096, kn=4864, vn=5632)
NVT = 53
NGB = 896


class Stat:
    def __init__(self, cx, name, n):
        self.t = cx.sb(name, [128, n], F32)
        self.o = 0
        self.n = n

    def get(self, w):
        assert self.o + w <= self.n
        ap = self.t[:, self.o:self.o + w]
        self.o += w
        return ap, T()


def bc(ap, shape):
    return ap.to_broadcast(list(shape))


def build_pre():
    cx = Ctx()
    P = cx.P
    x = cx.din("x", [TOK, D], F32)
    vT_d = cx.din("vT", [128, NVT], F32)
    gb_d = cx.din("gb", [1, NGB], F32)
    csm_d = cx.din("csm", [TOK, 64], F32)
    csa_d = cx.din("csa", [TOK, 128], F32)
    w_in = cx.din("w_in", [D, D_IN], F32)
    w_uq = cx.din("w_uq", [384, 1152], F32)
    w_ukv = cx.din("w_ukv", [256, 1536], F32)
    qkv = cx.dout("qkv", [TOK, QKV_COLS], BF16)
    cx.psum_banks()
    cx.ident()
    banks, bankT = cx.banks, cx.bankT
    st = Stat(cx, "stat", 1024)

    vT = cx.sb("vT", [128, NVT], F32)
    gb = cx.sb("gb", [128, NGB], F32)
    csm = cx.sb("csm", [128, NTC, 64], F32)
    csa = cx.sb("csa", [128, NTC, 128], F32)
    mhalf = cx.sb("mhalf", [128, 8], F32)
    tv, tgb, tcsm, tcsa, tmh = T(), T(), T(), T(), T()
    s0 = P.dsem("s0")
    P.dma("sp", vT[:], vT_d, s0, writes=[tv])
    P.dma("sp", gb[:], gb_d.to_broadcast([128, NGB]), s0, writes=[tgb])
    P.dma("sp", csm[:], csm_d.rearrange("(c p) n -> p c n", p=128), s0, writes=[tcsm])
    P.dma("sp", csa[:], csa_d.rearrange("(c p) n -> p c n", p=128), s0, writes=[tcsa])
    P.op("pool", lambda e: e.memset(mhalf[:], -0.5), writes=[tmh])
    A1T, tA1 = st.get(KD)
    P.op("dve", lambda e: e.scalar_tensor_tensor(A1T, vT[:, 16:32], 1.0, vT[:, 0:16], ALU.add, ALU.mult),
         reads=[tv], writes=[tA1])

    def rstd_of(ss, tss, w, div):
        ms, tms = st.get(w)
        rs, trs = st.get(w)
        P.op("pool", lambda e: e.tensor_scalar(ms, ss, 1.0 / div, EPS, ALU.mult, ALU.add), reads=[tss], writes=[tms])
        P.op("pool", lambda e: e.tensor_tensor(rs, ms, mhalf[:, 0:w], ALU.pow), reads=[tms, tmh], writes=[trs])
        return rs, trs

    hn = cx.sb("hn", [128, NTC, D], BF16)
    thn = [T() for _ in range(NTC)]
    xr = Ring(cx, "x", [128, D], F32, 2)
    junk = cx.sb("junk", [128, D], BF16)
    tjunk = T()
    for tc in range(NTC):
        xt, xT, xs = xr.next()
        P.dma("sp", xt[:], x[tc * 128:(tc + 1) * 128, :], xs, writes=[xT])
        ss, tss = st.get(1)
        P.op("act", lambda e, xt=xt, ss=ss: e.activation(junk[:], xt[:], AF.Square, accum_out=ss),
             reads=[xT], writes=[tjunk, tss])
        rs, trs = rstd_of(ss, tss, 1, D)
        P.op("dve", lambda e, xt=xt, rs=rs, tc=tc: e.tensor_scalar(hn[:, tc, :], xt[:], rs, None, ALU.mult),
             reads=[xT, trs], writes=[thn[tc]])

    h1T = cx.sb("h1T", [128, KD, TOK], BF16)
    th1T = T()
    for k in range(KD):
        bk = 6 + (k % 2)
        psv = banks[bk][:].bitcast(BF16)
        for tc in range(NTC):
            P.op("pe", lambda e, psv=psv, tc=tc, k=k: e.transpose(psv[:, tc * 128:(tc + 1) * 128],
                                                                   hn[:, tc, k * 128:(k + 1) * 128], cx.idb[:]),
                 reads=[thn[tc], cx.tid], writes=[bankT[bk]])
        P.op("act", lambda e, psv=psv, k=k: e.activation(h1T[:, k, :], psv, AF.Identity,
                                                          scale=A1T[:, k:k + 1], bias=vT[:, 32 + k:33 + k]),
             reads=[bankT[bk], tA1, tv], writes=[th1T])

    rawr = Ring(cx, "raw", [128, 1536], F32, 2, dma=False)
    sqr = Ring(cx, "sq", [128, 1536], F32, 2, dma=False)
    yr = Ring(cx, "y", [128, 1152], F32, 2, dma=False)
    tr_ = Ring(cx, "tmp", [128, 4, 256], F32, 2, dma=False)
    tmpTs = {}
    outr = Ring(cx, "ost", [128, 1152], BF16, 4)

    def evac(bl, want_sq=True):
        raw, rT, _ = rawr.next()
        sq, sT, _ = sqr.next()
        c = 0
        for bk, n in bl:
            P.op("act", lambda e, raw=raw, c=c, n=n, bk=bk: e.copy(raw[:, c:c + n], banks[bk][:, 0:n]),
                 reads=[bankT[bk]], writes=[rT])
            if want_sq:
                P.op("act", lambda e, sq=sq, c=c, n=n, bk=bk: e.activation(sq[:, c:c + n], banks[bk][:, 0:n], AF.Square),
                     reads=[bankT[bk]], writes=[sT])
            c += n
        return raw, rT, sq, sT

    def store(tc, ost, oT, osem, col0, n):
        P.dma("sp", qkv[tc * 128:(tc + 1) * 128, col0:col0 + n], ost[:, 0:n], osem, reads=[oT])

    def rope(yv, tY, ov, tO, cs, tcs, H, A):
        y5 = yv.rearrange("p h (a two d) -> p h a two d", a=A, two=2)
        o5 = ov.rearrange("p h (a two d) -> p h a two d", a=A, two=2)
        x1, x2 = y5[:, :, :, 0, :], y5[:, :, :, 1, :]
        shp = [128, H, A, 32]
        cos = bc(cs[:, 0:A * 32].rearrange("p (a d) -> p a d", a=A).unsqueeze(1), shp)
        sin = bc(cs[:, A * 32:2 * A * 32].rearrange("p (a d) -> p a d", a=A).unsqueeze(1), shp)
        tmp, tT, _ = tr_.next()
        n = H * A * 32
        tv_ = [tmp[:, i, 0:n].rearrange("p (h a d) -> p h a d", h=H, a=A) for i in range(4)]
        tTs = tmpTs.setdefault(id(tT), [T() for _ in range(4)])
        P.op("dve", lambda e: e.tensor_tensor(tv_[0], x1, cos, ALU.mult), reads=[tY, tcs], writes=[tTs[0]])
        P.op("dve", lambda e: e.tensor_tensor(tv_[1], x2, sin, ALU.mult), reads=[tY, tcs], writes=[tTs[1]])
        P.op("pool", lambda e: e.tensor_tensor(tv_[2], x2, cos, ALU.mult), reads=[tY, tcs], writes=[tTs[2]])
        P.op("pool", lambda e: e.tensor_tensor(tv_[3], x1, sin, ALU.mult), reads=[tY, tcs], writes=[tTs[3]])
        P.op("dve", lambda e: e.tensor_tensor(o5[:, :, :, 0, :], tv_[0], tv_[1], ALU.subtract),
             reads=[tTs[0], tTs[1]], writes=[tO])
        P.op("pool", lambda e: e.tensor_tensor(o5[:, :, :, 1, :], tv_[2], tv_[3], ALU.add),
             reads=[tTs[2], tTs[3]], writes=[tO])

    def heads_norm(raw3, rT, sq3, sT, H, Dh, g_ap, extra=None, div=None):
        ss, tss = st.get(H)
        P.op("dve", lambda e: e.tensor_reduce(ss, sq3, AX.X, ALU.add), reads=[sT], writes=[tss])
        if extra is not None:
            ex, tex = extra
            P.op("dve", lambda e: e.tensor_tensor(ss, ss, bc(ex, [128, H]), ALU.add), reads=[tss, tex], writes=[tss])
        rs, trs = rstd_of(ss, tss, H, div or Dh)
        y, yT, _ = yr.next()
        y3 = y[:, 0:H * Dh].rearrange("p (h d) -> p h d", h=H)
        P.op("dve", lambda e: e.tensor_tensor(y3, raw3, bc(rs.unsqueeze(2), [128, H, Dh]), ALU.mult),
             reads=[rT, trs], writes=[yT])
        return y3, yT, rs, trs

    aqn = cx.sb("aqn", [128, NTC, 384], BF16)
    akvn = cx.sb("akvn", [128, NTC, 256], BF16)
    kper = cx.sb("kper", [128, NTC, 64], F32)
    sspe, _ = st.get(NTC)
    taqn = [T() for _ in range(NTC)]
    takvn = [T() for _ in range(NTC)]
    tkper = [T() for _ in range(NTC)]
    tsspe = [T() for _ in range(NTC)]

    G_QQ, G_QK, G_GQ, G_GK, G_NQ, G_NK = 0, 192, 384, 512, 640, 768

    blocks = [(0, 704, [384, 320]), (704, 512, [512]), (1216, 512, [512]),
              (1728, 768, [384, 384]), (2496, 768, [384, 384]), (3264, 768, [384, 384])]
    win1 = cx.sb("win1", [128, KD, 768], BF16)
    win0 = hn[:].rearrange("p c d -> p (c d)")[:, 0:KD * 768].rearrange("p (k n) -> p k n", k=KD)
    wslots = [(win0, T(), P.dsem("s_win0")), (win1[:], T(), P.dsem("s_win1"))]
    it = 0
    for b, (c0, n, subs) in enumerate(blocks):
        wt, wT, ws = wslots[(b + 1) % 2]
        for kk in range(0, KD, 4):
            P.dma("pool", wt[:, kk:kk + 4, 0:n], w_in[kk * 128:(kk + 4) * 128, c0:c0 + n].rearrange("(k p) n -> p k n", p=128),
                  ws, writes=[wT] + (thn if (b + 1) % 2 == 0 else []))
        for tc in range(NTC):
            bset = (it % 2) * 3
            it += 1
            s0_ = 0
            bl = []
            for si, sn in enumerate(subs):
                bk = bset + si
                for k in range(KD):
                    P.op("pe", lambda e, bk=bk, sn=sn, k=k, tc=tc, wt=wt, s0_=s0_: e.matmul(
                        banks[bk][:, 0:sn], h1T[:, k, tc * 128:(tc + 1) * 128], wt[:, k, s0_:s0_ + sn],
                        start=(k == 0), stop=(k == KD - 1)), reads=[th1T, wT], writes=[bankT[bk]])
                bl.append((bk, sn))
                s0_ += sn
            if b == 0:
                raw, rT, sq, sT = evac(bl)
                ss, tss = st.get(2)
                P.op("dve", lambda e, ss=ss, sq=sq: e.tensor_reduce(ss[:, 0:1], sq[:, 0:384], AX.X, ALU.add), reads=[sT], writes=[tss])
                P.op("dve", lambda e, ss=ss, sq=sq: e.tensor_reduce(ss[:, 1:2], sq[:, 384:640], AX.X, ALU.add), reads=[sT, tss], writes=[tss])
                P.op("dve", lambda e, sq=sq, tc=tc: e.tensor_reduce(sspe[:, tc:tc + 1], sq[:, 640:704], AX.X, ALU.add),
                     reads=[sT], writes=[tsspe[tc]])
                rq, trq = rstd_of(ss[:, 0:1], tss, 1, 384)
                rk, trk = rstd_of(ss[:, 1:2], tss, 1, 256)
                P.op("dve", lambda e, raw=raw, rq=rq, tc=tc: e.tensor_scalar(aqn[:, tc, :], raw[:, 0:384], rq, None, ALU.mult),
                     reads=[rT, trq], writes=[taqn[tc]])
                P.op("dve", lambda e, raw=raw, rk=rk, tc=tc: e.tensor_scalar(akvn[:, tc, :], raw[:, 384:640], rk, None, ALU.mult),
                     reads=[rT, trk], writes=[takvn[tc]])
                y, yT, _ = yr.next()
                P.op("dve", lambda e, y=y, raw=raw: e.tensor_tensor(y[:, 0:64], raw[:, 640:704], gb[:, G_QK + 128:G_QK + 192], ALU.mult),
                     reads=[rT, tgb], writes=[yT])
                rope(y[:, 0:64].unsqueeze(1), yT, kper[:, tc, :].unsqueeze(1), tkper[tc], csm[:, tc, :], tcsm, 1, 1)
            elif b == 1:
                raw, rT, sq, sT = evac(bl)
                y3, yT, _, _ = heads_norm(raw[:, 0:512].rearrange("p (h d) -> p h d", h=4), rT,
                                          sq[:, 0:512].rearrange("p (h d) -> p h d", h=4), sT, 4, 128, None)
                P.op("pool", lambda e, y3=y3: e.tensor_tensor(y3, y3, bc(gb[:, G_GQ:G_GQ + 128].unsqueeze(1), [128, 4, 128]), ALU.mult),
                     reads=[yT, tgb], writes=[yT])
                ost, oT, osem = outr.next()
                rope(y3, yT, ost[:, 0:512].rearrange("p (h d) -> p h d", h=4), oT, csa[:, tc, :], tcsa, 4, 2)
                store(tc, ost, oT, osem, OFF["qg"], 512)
            elif b == 2:
                raw, rT, sq, sT = evac(bl)
                y3, yT, _, _ = heads_norm(raw[:, 0:256].rearrange("p (h d) -> p h d", h=2), rT,
                                          sq[:, 0:256].rearrange("p (h d) -> p h d", h=2), sT, 2, 128, None)
                P.op("pool", lambda e, y3=y3: e.tensor_tensor(y3, y3, bc(gb[:, G_GK:G_GK + 128].unsqueeze(1), [128, 2, 128]), ALU.mult),
                     reads=[yT, tgb], writes=[yT])
                ost, oT, osem = outr.next()
                rope(y3, yT, ost[:, 0:256].rearrange("p (h d) -> p h d", h=2), oT, csa[:, tc, :], tcsa, 2, 2)
                P.op("act", lambda e, ost=ost, raw=raw: e.copy(ost[:, 256:512], raw[:, 256:512]), reads=[rT], writes=[oT])
                store(tc, ost, oT, osem, OFF["kg"], 512)
            elif b in (3, 4):
                raw, rT, sq, sT = evac(bl)
                y3, yT, _, _ = heads_norm(raw[:, 0:768].rearrange("p (h d) -> p h d", h=6), rT,
                                          sq[:, 0:768].rearrange("p (h d) -> p h d", h=6), sT, 6, 128, None)
                g0 = G_NQ if b == 3 else G_NK
                ost, oT, osem = outr.next()
                P.op("pool", lambda e, y3=y3, ost=ost, g0=g0: e.tensor_tensor(
                    ost[:, 0:768].rearrange("p (h d) -> p h d", h=6), y3,
                    bc(gb[:, g0:g0 + 128].unsqueeze(1), [128, 6, 128]), ALU.mult), reads=[yT, tgb], writes=[oT])
                store(tc, ost, oT, osem, OFF["qn"] if b == 3 else OFF["kn"], 768)
            else:
                ost, oT, osem = outr.next()
                c = 0
                for bk, sn in bl:
                    P.op("act", lambda e, ost=ost, c=c, sn=sn, bk=bk: e.copy(ost[:, c:c + sn], banks[bk][:, 0:sn]),
                         reads=[bankT[bk]], writes=[oT])
                    c += sn
                store(tc, ost, oT, osem, OFF["vn"], 768)

    aqnT = cx.sb("aqnT", [128, 3, TOK], BF16)
    akvnT = cx.sb("akvnT", [128, 2, TOK], BF16)
    taqnT, takvnT = T(), T()
    i = 0
    for (src, tsrc, dst, tdst, nj, vcol) in ((aqn, taqn, aqnT, taqnT, 3, 48), (akvn, takvn, akvnT, takvnT, 2, 51)):
        for j in range(nj):
            bk = 6 + (i % 2)
            i += 1
            psv = banks[bk][:].bitcast(BF16)
            for tc in range(NTC):
                P.op("pe", lambda e, psv=psv, tc=tc, j=j, src=src: e.transpose(
                    psv[:, tc * 128:(tc + 1) * 128], src[:, tc, j * 128:(j + 1) * 128], cx.idb[:]),
                    reads=[tsrc[tc], cx.tid], writes=[bankT[bk]])
            P.op("act", lambda e, psv=psv, j=j, dst=dst, vcol=vcol: e.activation(
                dst[:, j, :], psv, AF.Identity, scale=vT[:, vcol + j:vcol + j + 1]),
                reads=[bankT[bk], tv], writes=[tdst])

    w1flat = win1[:].rearrange("p k n -> p (k n)")
    uq = w1flat[:, 0:3456].rearrange("p (k n) -> p k n", k=3)
    ukv = w1flat[:, 3456:3456 + 3072].rearrange("p (k n) -> p k n", k=2)
    tuq = tukv = wslots[1][1]
    suq = wslots[1][2]
    P.dma("pool", uq, w_uq.rearrange("(k p) n -> p k n", p=128), suq, writes=[tuq])
    P.dma("pool", ukv, w_ukv.rearrange("(k p) n -> p k n", p=128), suq, writes=[tukv])
    for tc in range(NTC):
        bset = (it % 2) * 3
        it += 1
        bl = []
        for si in range(3):
            bk = bset + si
            for k in range(3):
                P.op("pe", lambda e, bk=bk, si=si, k=k, tc=tc: e.matmul(
                    banks[bk][:, 0:384], aqnT[:, k, tc * 128:(tc + 1) * 128], uq[:, k, si * 384:(si + 1) * 384],
                    start=(k == 0), stop=(k == 2)), reads=[taqnT, tuq], writes=[bankT[bk]])
            bl.append((bk, 384))
        raw, rT, sq, sT = evac(bl)
        y3, yT, _, _ = heads_norm(raw[:, 0:1152].rearrange("p (h d) -> p h d", h=6), rT,
                                  sq[:, 0:1152].rearrange("p (h d) -> p h d", h=6), sT, 6, 192, None)
        P.op("pool", lambda e, y3=y3: e.tensor_tensor(y3, y3, bc(gb[:, G_QQ:G_QQ + 192].unsqueeze(1), [128, 6, 192]), ALU.mult),
             reads=[yT, tgb], writes=[yT])
        ost, oT, osem = outr.next()
        o3 = ost[:, 0:1152].rearrange("p (h d) -> p h d", h=6)
        P.op("act", lambda e, o3=o3, y3=y3: e.copy(o3[:, :, 0:128], y3[:, :, 0:128]), reads=[yT], writes=[oT])
        rope(y3[:, :, 128:192], yT, o3[:, :, 128:192], oT, csm[:, tc, :], tcsm, 6, 1)
        store(tc, ost, oT, osem, OFF["qm"], 1152)

    for tc in range(NTC):
        bset = (it % 2) * 3
        it += 1
        bl = []
        for si in range(3):
            bk = bset + si
            for k in range(2):
                P.op("pe", lambda e, bk=bk, si=si, k=k, tc=tc: e.matmul(
                    banks[bk][:, 0:512], akvnT[:, k, tc * 128:(tc + 1) * 128], ukv[:, k, si * 512:(si + 1) * 512],
                    start=(k == 0), stop=(k == 1)), reads=[takvnT, tukv], writes=[bankT[bk]])
            bl.append((bk, 512))
        raw, rT, sq, sT = evac(bl)
        raw3 = raw[:, 0:1536].rearrange("p (h d) -> p h d", h=6)
        sq3 = sq[:, 0:1536].rearrange("p (h d) -> p h d", h=6)
        y3, yT, rs, trs = heads_norm(raw3[:, :, 0:128], rT, sq3[:, :, 0:128], sT, 6, 128, None,
                                     extra=(sspe[:, tc:tc + 1], tsspe[tc]), div=192)
        ost, oT, osem = outr.next()
        o3 = ost[:, 0:1152].rearrange("p (h d) -> p h d", h=6)
        P.op("pool", lambda e, o3=o3, y3=y3: e.tensor_tensor(o3[:, :, 0:128], y3, bc(gb[:, G_QK:G_QK + 128].unsqueeze(1), [128, 6, 128]), ALU.mult),
             reads=[yT, tgb], writes=[oT])
        P.op("dve", lambda e, o3=o3, rs=rs, tc=tc: e.tensor_tensor(
            o3[:, :, 128:192], bc(kper[:, tc, :].unsqueeze(1), [128, 6, 64]), bc(rs.unsqueeze(2), [128, 6, 64]), ALU.mult),
            reads=[tkper[tc], trs], writes=[oT])
        store(tc, ost, oT, osem, OFF["km"], 1152)
        ost2, oT2, osem2 = outr.next()
        P.op("act", lambda e, ost2=ost2, raw3=raw3: e.copy(ost2[:, 0:768].rearrange("p (h d) -> p h d", h=6), raw3[:, :, 128:256]),
             reads=[rT], writes=[oT2])
        store(tc, ost2, oT2, osem2, OFF["vm"], 768)

    P.final_dsems = outr.ds
    return cx.finish()


def colT(v):
    return np.ascontiguousarray(np.asarray(v, np.float32).reshape(-1, 128).T)


def rope_tables():
    inv = (10000.0 ** (-np.arange(0, 64, 2, dtype=np.float32) / np.float32(64))).astype(np.float32)
    t = np.arange(SEQ)
    def cs(pos):
        ang = pos.astype(np.float32)[:, None] * inv[None, :]
        return np.cos(ang).astype(np.float32), np.sin(ang).astype(np.float32)
    cm, sm = cs(t)
    cr, sr = cs(t // 64)
    cc, sc = cs(t % 64)
    csm = np.concatenate([cm, sm], axis=1)
    csa = np.concatenate([cr, cc, sr, sc], axis=1)
    return np.ascontiguousarray(csm), np.ascontiguousarray(csa)


def pre_inputs(inp, l, x_full, mod, tabs):
    csm, csa = tabs
    m = mod[l].reshape(6, D)
    vT = np.concatenate([colT(inp["norm1_g"][l]), colT(m[1]), colT(m[0]),
                         colT(inp["mla_q_norm_g"][l]), colT(inp["mla_kv_norm_g"][l])], axis=1)
    gb = np.concatenate([inp["mla_qk_q_g"][l], inp["mla_qk_k_g"][l], inp["gqa_q_g"][l], inp["gqa_k_g"][l],
                         inp["na_q_g"][l], inp["na_k_g"][l]]).reshape(1, -1).astype(np.float32)
    maps = []
    for j in range(NCORES):
        sl = slice(j * TOK, (j + 1) * TOK)
        maps.append({"x": np.ascontiguousarray(x_full[sl]), "vT": vT, "gb": gb, "csm": csm[sl], "csa": csa[sl],
                     "w_in": inp["w_in"][l], "w_uq": inp["mla_w_uq"][l], "w_ukv": inp["mla_w_ukv"][l]})
    return maps


NPIECE = 4
KPB = 16
NHALO = 14
NBT = 29
NEG = -30000.0


def na_slots(p):
    d = [-2, -1, 0, 1, 2]
    if p == 0:
        d.append(3)
    if p == 7:
        d.append(-3)
    return d


def na_tile_index(p, si):
    if 2 <= p <= 5:
        return si
    sp = {0: 0, 1: 1, 6: 2, 7: 3}[p]
    return 5 + sp * 6 + si


def build_attn():
    cx = Ctx()
    P = cx.P
    qa = cx.din("qa", [10, 128, TOK], BF16)
    qb_ = cx.din("qb", [6, 64, TOK], BF16)
    ka = cx.din("ka", [10, 128, SEQ], BF16)
    kb_ = cx.din("kb", [6, 64, SEQ], BF16)
    va = cx.din("va", [10, NPIECE, 128, KPB * 130], BF16)
    qn = cx.din("qn", [6, 128, TOK], BF16)
    kn = cx.din("kn", [6, 128, NHALO * 128], BF16)
    vn = cx.din("vn", [6, 128, NHALO * 130], BF16)
    bn = cx.din("bn", [6, 128, NBT * 128], F32)
    oT_d = cx.dout("oT", [10, 128, TOK], F32)
    rs_d = cx.dout("rs", [1, 10 * TOK], F32)
    on_d = cx.dout("on", [TOK, 768], F32)
    cx.psum_banks()
    banks, bankT = cx.banks, cx.bankT

    qr = Ring(cx, "q", [128, 2, TOK], BF16, 2)
    kr = Ring(cx, "k", [128, 2, KPB * 128], BF16, 3)
    vr = Ring(cx, "v", [128, KPB * 130], BF16, 3)
    ptr = Ring(cx, "pt", [128, 512], BF16, 4, dma=False)
    racc = Ring(cx, "racc", [128, 512], F32, 4, dma=False)
    otr = Ring(cx, "ot", [128, 512], F32, 3)
    rsr = Ring(cx, "rsst", [1, 512], F32, 2)
    onesf = cx.sb("onesf", [128, 1], F32)
    tones = T()
    P.op("pool", lambda e: e.memset(onesf[:], 1.0), writes=[tones])

    units = []
    for hd in range(10):
        mla = hd < 6
        scale = (192.0 if mla else 128.0) ** -0.5
        hq = dict(loaded=False)
        ra = [racc.next() for _ in range(2)]
        for pc in range(NPIECE):
            pend = dict(hd=hd, pc=pc, loaded=False)
            for kb in range(KPB):
                for qb in range(2):
                    units.append(dict(hd=hd, mla=mla, scale=scale, hq=hq, pc=pc, kb=kb, qb=qb, pend=pend, ra=ra[qb],
                                      first=(pc == 0 and kb == 0), last=(pc == NPIECE - 1 and kb == KPB - 1)))

    def load_piece(u):
        pend = u["pend"]
        hd, pc, mla = u["hd"], u["pc"], u["mla"]
        hq = u["hq"]
        if not hq["loaded"]:
            qt, qT, qs = qr.next()
            P.dma("sp", qt[:, 0, :], qa[hd], qs, writes=[qT])
            if mla:
                P.dma("sp", qt[0:64, 1, :], qb_[hd], qs, writes=[qT])
            hq.update(loaded=True, qt=qt, qT=qT)
        if pend["loaded"]:
            return
        kt, kT, ks = kr.next()
        vt, vT_, vs = vr.next()
        P.dma("sp", kt[:, 0, :], ka[hd, :, pc * 2048:(pc + 1) * 2048], ks, writes=[kT])
        if mla:
            P.dma("sp", kt[0:64, 1, :], kb_[hd, :, pc * 2048:(pc + 1) * 2048], ks, writes=[kT])
        P.dma("pool", vt[:], va[hd, pc], vs, writes=[vT_])
        pend.update(loaded=True, kt=kt, kT=kT, vt=vt, vT=vT_)

    def QK(i, u):
        load_piece(u)
        pd = u["pend"]
        sb_ = 4 + (i % 3)
        kt, qt, kb, qb, mla = pd["kt"], u["hq"]["qt"], u["kb"], u["qb"], u["mla"]
        P.op("pe", lambda e: e.matmul(banks[sb_][:, :], kt[:, 0, kb * 128:(kb + 1) * 128], qt[:, 0, qb * 512:(qb + 1) * 512],
                                      start=True, stop=not mla), reads=[pd["kT"], u["hq"]["qT"]], writes=[bankT[sb_]])
        if mla:
            P.op("pe", lambda e: e.matmul(banks[sb_][:, :], kt[0:64, 1, kb * 128:(kb + 1) * 128], qt[0:64, 1, qb * 512:(qb + 1) * 512],
                                          start=False, stop=True), reads=[pd["kT"], u["hq"]["qT"]], writes=[bankT[sb_]])
        pt, pT, _ = ptr.next()
        sc = u["scale"]
        P.op("act", lambda e: e.activation(pt[:], banks[sb_][:, :], AF.Exp, scale=sc), reads=[bankT[sb_]], writes=[pT])
        u["pt"], u["pT"] = pt, pT

    def PV(i, u):
        pd = u["pend"]
        hd, qb, kb = u["hd"], u["qb"], u["kb"]
        ob = (hd % 2) * 2 + qb
        pt, pT, vt = u["pt"], u["pT"], pd["vt"]
        P.op("pe", lambda e: e.matmul(banks[ob][:, :], vt[:, kb * 130:kb * 130 + 128], pt[:], start=u["first"], stop=u["last"]),
             reads=[pT, pd["vT"]], writes=[bankT[ob]])
        rt_, rT_, _ = u["ra"]
        if u["first"]:
            P.op("dve", lambda e: e.tensor_copy(rt_[:], pt[:]), reads=[pT], writes=[rT_])
        else:
            P.op("dve", lambda e: e.tensor_tensor(rt_[:], rt_[:], pt[:], ALU.add), reads=[pT, rT_], writes=[rT_])
        if u["last"]:
            ot, oT, osem = otr.next()
            P.op("act", lambda e: e.copy(ot[:], banks[ob][:, :]), reads=[bankT[ob]], writes=[oT])
            P.dma("sp", oT_d[hd, :, qb * 512:(qb + 1) * 512], ot[:], osem, reads=[oT])
            P.op("pe", lambda e: e.matmul(banks[7][0:1, :], onesf[:], rt_[:], start=True, stop=True), reads=[rT_, tones], writes=[bankT[7]])
            rs, rsT, rss = rsr.next()
            P.op("dve", lambda e: e.tensor_copy(rs[:], banks[7][0:1, :]), reads=[bankT[7]], writes=[rsT])
            P.dma("sp", rs_d[0:1, hd * TOK + qb * 512:hd * TOK + (qb + 1) * 512], rs[:], rss, reads=[rsT])

    n = len(units)
    LOOK = 2
    for i in range(n + LOOK):
        if i < n:
            QK(i, units[i])
        if i - LOOK >= 0:
            PV(i - LOOK, units[i - LOOK])

    knr = Ring(cx, "kn", [128, NHALO * 128], BF16, 2)
    vnr = Ring(cx, "vn", [128, NHALO * 130], BF16, 2)
    bnr = Ring(cx, "bn", [128, NBT * 128], F32, 2)
    snr = Ring(cx, "sn", [128, 768], F32, 2, dma=False)
    pnr = Ring(cx, "pn", [128, 768], BF16, 2, dma=False)
    osr = Ring(cx, "os", [128, 8, 128], F32, 2)
    rinv = cx.sb("rinv", [128, 64], F32)
    rcnt = [0]
    o_v = on_d.rearrange("(a p) c -> p a c", p=128)
    scale = 128.0 ** -0.5
    ai = 0
    sbank = [0]
    for h in range(6):
        qt, qT, qs = qr.next()
        P.dma("sp", qt[:, 0, :], qn[h], qs, writes=[qT])
        kt, kT, ks = knr.next()
        vt, vT_, vs = vnr.next()
        bt, bT, bs = bnr.next()
        P.dma("sp", kt[:], kn[h], ks, writes=[kT])
        P.dma("sp", vt[:], vn[h], vs, writes=[vT_])
        P.dma("pool", bt[:], bn[h], bs, writes=[bT])
        ost, oT, osem = osr.next()
        for p in range(NTC):
            slots = na_slots(p)
            ns = len(slots)
            sA, sB = 4 + (sbank[0] % 2) * 2, 5 + (sbank[0] % 2) * 2
            sbank[0] += 1
            for si, dl in enumerate(slots):
                sb_, c0 = (sA, si * 128) if si < 4 else (sB, (si - 4) * 128)
                hc = p + dl + 3
                P.op("pe", lambda e, sb_=sb_, c0=c0, kt=kt, qt=qt, hc=hc, p=p: e.matmul(
                    banks[sb_][:, c0:c0 + 128], kt[:, hc * 128:(hc + 1) * 128], qt[:, 0, p * 128:(p + 1) * 128],
                    start=True, stop=True, skip_group_check=True), reads=[kT, qT], writes=[bankT[sb_]])
            sn, sT, _ = snr.next()
            pn, pT, _ = pnr.next()
            for (sb_, s0, s1) in ((sA, 0, min(ns, 4)), (sB, 4, ns)):
                if s1 <= s0:
                    continue
                for si in range(s0, s1):
                    ti = na_tile_index(p, si)
                    c0 = (si - s0) * 128
                    P.op("dve", lambda e, sn=sn, si=si, sb_=sb_, c0=c0, bt=bt, ti=ti: e.scalar_tensor_tensor(
                        sn[:, si * 128:(si + 1) * 128], banks[sb_][:, c0:c0 + 128], scale, bt[:, ti * 128:(ti + 1) * 128],
                        ALU.mult, ALU.add), reads=[bankT[sb_], bT], writes=[sT])
            P.op("act", lambda e, pn=pn, sn=sn, ns=ns: e.activation(pn[:, 0:ns * 128], sn[:, 0:ns * 128], AF.Exp),
                 reads=[sT], writes=[pT])
            bk = ai % 4
            ai += 1
            for si, dl in enumerate(slots):
                hc = p + dl + 3
                P.op("pe", lambda e, bk=bk, pn=pn, si=si, vt=vt, hc=hc, ns=ns: e.matmul(
                    banks[bk][:, 0:129], pn[:, si * 128:(si + 1) * 128], vt[:, hc * 130:hc * 130 + 129],
                    start=(si == 0), stop=(si == ns - 1), skip_group_check=True), reads=[pT, vT_], writes=[bankT[bk]])
            ri = rinv[:, rcnt[0] % 64:rcnt[0] % 64 + 1]
            rcnt[0] += 1
            tri = T()
            P.op("dve", lambda e, ri=ri, bk=bk: e.reciprocal(ri, banks[bk][:, 128:129]), reads=[bankT[bk]], writes=[tri])
            P.op("dve", lambda e, ri=ri, bk=bk, ost=ost, p=p: e.tensor_scalar(ost[:, p, :], banks[bk][:, 0:128], ri, None, ALU.mult),
                 reads=[bankT[bk], tri], writes=[oT])
        P.dma("sp", o_v[:, :, h * 128:(h + 1) * 128], ost[:], osem, reads=[oT])

    P.final_dsems = osr.ds + otr.ds + rsr.ds
    return cx.finish()


def na_bias_tiles(rpb_l, core):
    H = 6
    out = np.full((H, NBT, 128, 128), NEG, np.float32)

    def tile(c, kc_):
        qi = np.arange(128)
        r = 2 * c + qi // 64
        cq = qi % 64
        ki = np.arange(128)
        kr = 2 * kc_ + ki // 64
        kcol = ki % 64
        rs = np.clip(r - 4, 0, 128 - 8)
        cs = np.clip(cq - 8, 0, 64 - 16)
        valid = ((kr[:, None] >= rs[None, :]) & (kr[:, None] < rs[None, :] + 8) &
                 (kcol[:, None] >= cs[None, :]) & (kcol[:, None] < cs[None, :] + 16))
        dr = kr[:, None] - r[None, :] + 7
        dc = np.clip(kcol[:, None] - cq[None, :] + 15, 0, 30)
        valid &= (dr >= 0) & (dr <= 14) & (kc_ >= 0) & (kc_ < 64)
        return valid, np.clip(dr, 0, 14), dc

    def fill(ti, c, kc_):
        valid, dr, dc = tile(c, kc_)
        for h in range(H):
            vals = rpb_l[h][dr, dc]
            out[h, ti] = np.where(valid, vals, np.float32(NEG))

    for si, dl in enumerate([-2, -1, 0, 1, 2]):
        fill(si, 10, 10 + dl)
    for p in (0, 1, 6, 7):
        c = core * 8 + p
        for si, dl in enumerate(na_slots(p)):
            fill(na_tile_index(p, si), c, c + dl)
    return np.ascontiguousarray(out.transpose(0, 2, 1, 3).reshape(H, 128, NBT * 128))


def attn_inputs(inp, l, qkv_all):
    q = qkv_all
    def cols(name, n):
        return q[:, OFF[name]:OFF[name] + n]
    qm = cols("qm", 1152).reshape(SEQ, 6, 192)
    km = cols("km", 1152).reshape(SEQ, 6, 192)
    vm = cols("vm", 768).reshape(SEQ, 6, 128)
    qg = cols("qg", 512).reshape(SEQ, 4, 128)
    kg = cols("kg", 256).reshape(SEQ, 2, 128)
    vg = cols("vg", 256).reshape(SEQ, 2, 128)
    qn_ = cols("qn", 768).reshape(SEQ, 6, 128)
    kn_ = cols("kn", 768).reshape(SEQ, 6, 128)
    vn_ = cols("vn", 768).reshape(SEQ, 6, 128)
    qa_all = np.concatenate([qm[:, :, 0:128], qg], axis=1).transpose(1, 2, 0)
    qb_all = qm[:, :, 128:192].transpose(1, 2, 0)
    ka_all = np.ascontiguousarray(np.concatenate([km[:, :, 0:128], kg[:, [0, 0, 1, 1], :]], axis=1).transpose(1, 2, 0))
    kb_all = np.ascontiguousarray(km[:, :, 128:192].transpose(1, 2, 0))
    v_all = np.concatenate([vm, vg[:, [0, 0, 1, 1], :]], axis=1)
    va = np.zeros((10, NPIECE, 128, KPB, 130), NPBF)
    va[..., 0:128] = v_all.reshape(NPIECE, KPB, 128, 10, 128).transpose(3, 0, 2, 1, 4)
    va[..., 128] = 1.0
    va = va.reshape(10, NPIECE, 128, KPB * 130)
    qn_all = qn_.transpose(1, 2, 0)
    maps = []
    for j in range(NCORES):
        sl = slice(j * TOK, (j + 1) * TOK)
        knh = np.zeros((6, 128, NHALO, 128), NPBF)
        vnh = np.zeros((6, 128, NHALO, 130), NPBF)
        for hc in range(NHALO):
            c = j * 8 + hc - 3
            if 0 <= c < 64:
                blk = slice(c * 128, (c + 1) * 128)
                knh[:, :, hc, :] = kn_[blk].transpose(1, 2, 0)
                vnh[:, :, hc, 0:128] = vn_[blk].transpose(1, 0, 2)
                vnh[:, :, hc, 128] = 1.0
        maps.append({
            "qa": np.ascontiguousarray(qa_all[:, :, sl]), "qb": np.ascontiguousarray(qb_all[:, :, sl]),
            "ka": ka_all, "kb": kb_all, "va": va,
            "qn": np.ascontiguousarray(qn_all[:, :, sl]),
            "kn": knh.reshape(6, 128, NHALO * 128), "vn": vnh.reshape(6, 128, NHALO * 130),
            "bn": na_bias_tiles(inp["na_rpb"][l], j),
        })
    return maps


CAP = 128
NR = 4
NSLOT = 4
BIG = 1.0e4


class Slot:
    def __init__(self, cx, arena, i):
        self.v = arena[:, i, :]
        self.Tw = T()
        self.Th = [T(), T()]
        self.sem = cx.P.dsem(f"s_ar{i}")
        self.hsem = [cx.P.dsem(f"s_ar{i}h0"), cx.P.dsem(f"s_ar{i}h1")]

    def wW(self):
        return [self.Tw, self.Th[0], self.Th[1]]

    def rW(self):
        return [self.Tw]

    def half(self, i):
        return self.v.bitcast(F32)[:, i * D:(i + 1) * D]

    def wH(self, i):
        return [self.Th[i], self.Tw]

    def rH(self, i):
        return [self.Th[i]]


def build_post():
    cx = Ctx()
    P = cx.P
    x_d = cx.din("x", [TOK, D], F32)
    o_d = cx.din("o", [TOK, D], F32)
    rs_d = cx.din("rs", [TOK, 16], F32)
    vT_d = cx.din("vT", [128, KD], F32)
    rows = cx.din("rows", [5, D], F32)
    w_out = cx.din("w_out", [D, D], F32)
    wr_d = cx.din("wr", [D, 36], F32)
    rb_d = cx.din("rb", [1, 36], F32)
    wg_d = cx.din("wg", [NE, D, DE], F32)
    wu_d = cx.din("wu", [NE, D, DE], F32)
    wd_d = cx.din("wd", [NE, DE, D], F32)
    xo = cx.dout("xo", [TOK, D], F32)
    cx.psum_banks()
    cx.ident()
    banks, bankT = cx.banks, cx.bankT
    st = Stat(cx, "stat", 384)
    s0 = P.dsem("s0")

    vT = cx.sb("vT", [128, KD], F32)
    tv = T()
    P.dma("sp", vT[:], vT_d, s0, writes=[tv])
    mhalf = cx.sb("mhalf", [128, 8], F32)
    tmh = T()
    P.op("pool", lambda e: e.memset(mhalf[:], -0.5), writes=[tmh])

    def rstd_of(ss, tss, w, div):
        ms, tms = st.get(w)
        rs, trs = st.get(w)
        P.op("pool", lambda e: e.tensor_scalar(ms, ss, 1.0 / div, EPS, ALU.mult, ALU.add), reads=[tss], writes=[tms])
        P.op("pool", lambda e: e.tensor_tensor(rs, ms, mhalf[:, 0:w], ALU.pow), reads=[tms, tmh], writes=[trs])
        return rs, trs

    xs = cx.sb("xs", [128, NTC, D], F32)
    tx = [T() for _ in range(NTC)]
    sx = P.dsem("sx")
    for tc in range(NTC):
        P.dma("sp", xs[:, tc, :], x_d[tc * 128:(tc + 1) * 128, :], sx, writes=[tx[tc]])
    gbc = cx.sb("gbc", [128, D], F32)
    tgbc = T()
    sg = P.dsem("sg")
    P.dma("sp", gbc[:], rows[0:1, :].to_broadcast([128, D]), sg, writes=[tgbc])

    arena = cx.sb("arena", [128, NSLOT, 8192], BF16)
    SL = [Slot(cx, arena, i) for i in range(NSLOT)]
    hb = cx.sb("hb", [128, NTC, D], BF16)
    thb = [T() for _ in range(NTC)]
    xer = Ring(cx, "xe", [128, KD, CAP], BF16, 2, dma=False)
    junk = xer.bufs[0][:].rearrange("p k s -> p (k s)")
    tjunk = xer.ts[0]

    rsum = cx.sb("rsum", [128, NTC, 16], F32)
    trsum = T()
    P.dma("sp", rsum[:], rs_d.rearrange("(c p) h -> p c h", p=128), s0, writes=[trsum])
    GH = ((0, 6), (6, 10), (10, 16))
    for tc in range(NTC):
        hi = tc % 2
        ot = SL[2].half(hi)
        P.dma("sp", ot, o_d[tc * 128:(tc + 1) * 128, :], SL[2].hsem[hi], writes=SL[2].wH(hi))
        ss, tss = st.get(16)
        for h in range(16):
            P.op("act", lambda e, ot=ot, h=h, ss=ss: e.activation(junk[:, h * 128:(h + 1) * 128], ot[:, h * 128:(h + 1) * 128], AF.Square,
                                                                  accum_out=ss[:, h:h + 1]),
                 reads=SL[2].rH(hi), writes=[tjunk, tss])
        rinv, trinv = st.get(16)
        P.op("dve", lambda e, rinv=rinv, tc=tc: e.reciprocal(rinv, rsum[:, tc, :]), reads=[trsum], writes=[trinv])
        P.op("dve", lambda e, ss=ss, rinv=rinv: e.tensor_tensor(ss, ss, rinv, ALU.mult), reads=[tss, trinv], writes=[tss])
        P.op("dve", lambda e, ss=ss, rinv=rinv: e.tensor_tensor(ss, ss, rinv, ALU.mult), reads=[tss, trinv], writes=[tss])
        sg_, tsg = st.get(3)
        for gi, (a, b) in enumerate(GH):
            P.op("dve", lambda e, sg_=sg_, ss=ss, gi=gi, a=a, b=b: e.tensor_reduce(sg_[:, gi:gi + 1], ss[:, a:b], AX.X, ALU.add),
                 reads=[tss], writes=[tsg])
        ms, tms = st.get(3)
        for gi, (a, b) in enumerate(GH):
            P.op("pool", lambda e, ms=ms, sg_=sg_, gi=gi, a=a, b=b: e.tensor_scalar(ms[:, gi:gi + 1], sg_[:, gi:gi + 1], 1.0 / ((b - a) * 128), EPS, ALU.mult, ALU.add),
                 reads=[tsg], writes=[tms])
        rs, trs = st.get(3)
        P.op("pool", lambda e, rs=rs, ms=ms: e.tensor_tensor(rs, ms, mhalf[:, 0:3], ALU.pow), reads=[tms, tmh], writes=[trs])
        for gi, (a, b) in enumerate(GH):
            P.op("dve", lambda e, rinv=rinv, rs=rs, gi=gi, a=a, b=b: e.tensor_scalar(rinv[:, a:b], rinv[:, a:b], rs[:, gi:gi + 1], None, ALU.mult),
                 reads=[trinv, trs], writes=[trinv])
        P.op("dve", lambda e, ot=ot, rinv=rinv, tc=tc: e.tensor_tensor(hb[:, tc, :].rearrange("p (h d) -> p h d", h=16), ot.rearrange("p (h d) -> p h d", h=16),
                                                                       bc(rinv.unsqueeze(2), [128, 16, 128]), ALU.mult),
             reads=SL[2].rH(hi) + [trinv], writes=[thb[tc]])
    oTv = arena[:, 0:2, :].rearrange("p s n -> p (s n)").rearrange("p (k t) -> p k t", k=KD)
    for k in range(KD):
        bk = 6 + (k % 2)
        psv = banks[bk][:].bitcast(BF16)
        for tc in range(NTC):
            P.op("pe", lambda e, psv=psv, tc=tc, k=k: e.transpose(psv[:, tc * 128:(tc + 1) * 128], hb[:, tc, k * 128:(k + 1) * 128], cx.idb[:]),
                 reads=[thb[tc], cx.tid], writes=[bankT[bk]])
        P.op("act", lambda e, psv=psv, k=k: e.activation(oTv[:, k, :], psv, AF.Identity, scale=vT[:, k:k + 1]),
             reads=[bankT[bk], tv], writes=SL[k // 8].wW())

    scr = Ring(cx, "scr", [128, 512], F32, 1, dma=False)
    it = 0
    wv = SL[2].v.rearrange("p (k n) -> p k n", k=KD)
    for cb in range(4):
        for kk in range(0, KD, 4):
            P.dma("pool", wv[:, kk:kk + 4, :], w_out[kk * 128:(kk + 4) * 128, cb * 512:(cb + 1) * 512].rearrange("(k p) n -> p k n", p=128),
                  SL[2].sem, writes=SL[2].wW())
        for tc in range(NTC):
            bk = it % 4
            it += 1
            for k in range(KD):
                P.op("pe", lambda e, bk=bk, k=k, tc=tc: e.matmul(banks[bk][:, :], oTv[:, k, tc * 128:(tc + 1) * 128], wv[:, k, :],
                                                                 start=(k == 0), stop=(k == KD - 1)),
                     reads=SL[0].rW() + SL[1].rW() + SL[2].rW(), writes=[bankT[bk]])
            tm, tmT, _ = scr.next()
            P.op("dve", lambda e, tm=tm, bk=bk, cb=cb: e.tensor_tensor(tm[:], banks[bk][:, :], gbc[:, cb * 512:(cb + 1) * 512], ALU.mult),
                 reads=[bankT[bk], tgbc], writes=[tmT])
            P.op("pool", lambda e, tm=tm, tc=tc, cb=cb: e.tensor_tensor(xs[:, tc, cb * 512:(cb + 1) * 512], xs[:, tc, cb * 512:(cb + 1) * 512], tm[:], ALU.add),
                 reads=[tmT, tx[tc]], writes=[tx[tc]])

    A2b = SL[2].v.bitcast(F32)
    P.dma("sp", A2b[:, 0:D], rows[1:2, :].to_broadcast([128, D]), SL[2].sem, writes=SL[2].wW())
    P.dma("sp", A2b[:, D:2 * D], rows[3:4, :].to_broadcast([128, D]), SL[2].sem, writes=SL[2].wW())
    scb = SL[0].half(1)
    P.dma("sp", scb, rows[2:3, :].to_broadcast([128, D]), SL[0].hsem[1], writes=SL[0].wH(1))
    P.op("dve", lambda e: e.scalar_tensor_tensor(A2b[:, 0:D], scb, 1.0, A2b[:, 0:D], ALU.add, ALU.mult),
         reads=SL[0].rH(1) + SL[2].rW(), writes=SL[2].wW())
    wr = cx.sb("wr", [128, KD, 36], F32)
    rbb = cx.sb("rbb", [128, 36], F32)
    twr, trb = T(), T()
    P.dma("sp", wr[:], wr_d.rearrange("(k p) n -> p k n", p=128), s0, writes=[twr])
    P.dma("sp", rbb[:], rb_d.to_broadcast([128, 36]), s0, writes=[trb])
    ybig = cx.sb("ybig", [128, 2, D], BF16)
    h2T = ybig[:].rearrange("p a d -> p (a d)").bitcast(F32).rearrange("p (k t) -> p k t", k=KD)
    th2T = T()
    ybT = [T(), T()]
    A_f = cx.sb("A_f", [128, NTC, 32], F32)
    A_b = cx.sb("A_b", [128, NTC, 32], BF16)
    Wt_f = cx.sb("Wt_f", [128, NTC, 32], F32)
    rank_f = cx.sb("rank_f", [128, NTC, 32], F32)
    tA = [T() for _ in range(NTC)]
    tW = [T() for _ in range(NTC)]
    tR = [T() for _ in range(NTC)]
    rt = cx.sb("rt", [128, 256], F32)
    tr = T()
    for tc in range(NTC):
        ss, tss = st.get(1)
        P.op("act", lambda e, tc=tc, ss=ss: e.activation(junk[:], xs[:, tc, :], AF.Square, accum_out=ss),
             reads=[tx[tc]], writes=[tjunk, tss])
        rs, trs = rstd_of(ss, tss, 1, D)
        hi = tc % 2
        hf = SL[0].half(hi)
        P.op("dve", lambda e, hf=hf, tc=tc, rs=rs: e.scalar_tensor_tensor(hf, xs[:, tc, :], rs, A2b[:, 0:D], ALU.mult, ALU.mult),
             reads=[tx[tc], trs] + SL[2].rW(), writes=SL[0].wH(hi))
        P.op("pool", lambda e, hf=hf: e.tensor_tensor(hf, hf, A2b[:, D:2 * D], ALU.add), reads=SL[0].rH(hi) + SL[2].rW(), writes=SL[0].wH(hi))
        P.op("act", lambda e, hf=hf, tc=tc: e.copy(hb[:, tc, :], hf), reads=SL[0].rH(hi), writes=[thb[tc]])
        for k4 in range(4):
            bk = 4 + (k4 % 2)
            for kk in range(4):
                k = k4 * 4 + kk
                P.op("pe", lambda e, bk=bk, kk=kk, k=k, hf=hf: e.transpose(banks[bk][:, kk * 128:(kk + 1) * 128], hf[:, k * 128:(k + 1) * 128], cx.idf[:]),
                     reads=SL[0].rH(hi) + [cx.tid], writes=[bankT[bk]])
            P.op("act", lambda e, bk=bk, k4=k4: e.copy(h2T[:, k4 * 4:(k4 + 1) * 4, :], banks[bk][:, :].rearrange("p (k t) -> p k t", k=4)),
                 reads=[bankT[bk]], writes=[th2T])
        for k in range(KD):
            P.op("pe", lambda e, k=k: e.matmul(banks[6][:, 0:36], h2T[:, k, :], wr[:, k, :], start=(k == 0), stop=(k == KD - 1)),
                 reads=[th2T, twr], writes=[bankT[6]])
        r = rt
        lg, gmax, ngmax, gsel, gexp, gsum, pg = r[:, 0:36], r[:, 36:37], r[:, 37:38], r[:, 38:42], r[:, 42:46], r[:, 46:47], r[:, 47:48]
        pen, em, m1, oh1 = r[:, 48:52], r[:, 52:84], r[:, 84:85], r[:, 88:120]
        em2, m2, oh2 = r[:, 120:152], r[:, 152:153], r[:, 160:192]
        nm1, w2, den, rden, wt1, wt2 = r[:, 192:193], r[:, 193:194], r[:, 194:195], r[:, 195:196], r[:, 196:197], r[:, 197:198]
        dv = lambda fn, rd=(), wr_=(): P.op("dve", fn, reads=[tr] + list(rd), writes=[tr] + list(wr_))
        dv(lambda e: e.tensor_tensor(lg, banks[6][:, 0:36], rbb[:], ALU.add), rd=[bankT[6], trb])
        dv(lambda e: e.tensor_reduce(gmax, lg[:, 0:4], AX.X, ALU.max))
        dv(lambda e: e.tensor_scalar(ngmax, gmax, -1.0, None, ALU.mult))
        dv(lambda e: e.tensor_scalar(gsel, lg[:, 0:4], gmax, None, ALU.is_ge))
        P.op("act", lambda e: e.activation(gexp, lg[:, 0:4], AF.Exp, bias=ngmax, accum_out=gsum), reads=[tr], writes=[tr])
        dv(lambda e: e.reciprocal(pg, gsum))
        dv(lambda e: e.tensor_scalar(pen, gsel, BIG, -BIG, ALU.mult, ALU.add))
        dv(lambda e: e.tensor_tensor(em.rearrange("p (g j) -> p g j", g=4), lg[:, 4:36].rearrange("p (g j) -> p g j", g=4),
                                     bc(pen.unsqueeze(2), [128, 4, 8]), ALU.add))
        dv(lambda e: e.tensor_reduce(m1, em, AX.X, ALU.max))
        dv(lambda e: e.tensor_scalar(oh1, em, m1, None, ALU.is_ge))
        dv(lambda e: e.scalar_tensor_tensor(em2, oh1, -BIG, em, ALU.mult, ALU.add))
        dv(lambda e: e.tensor_reduce(m2, em2, AX.X, ALU.max))
        dv(lambda e: e.tensor_scalar(oh2, em2, m2, None, ALU.is_ge))
        dv(lambda e: e.tensor_scalar(nm1, m1, -1.0, None, ALU.mult))
        P.op("act", lambda e: e.activation(w2, m2, AF.Exp, bias=nm1), reads=[tr], writes=[tr])
        dv(lambda e: e.tensor_scalar(den, w2, 1.0, None, ALU.add))
        dv(lambda e: e.reciprocal(rden, den))
        dv(lambda e: e.tensor_tensor(wt1, pg, rden, ALU.mult))
        dv(lambda e: e.tensor_tensor(wt2, pg, wt1, ALU.subtract))
        dv(lambda e, tc=tc: e.tensor_tensor(A_f[:, tc, :], oh1, oh2, ALU.add), wr_=[tA[tc]])
        dv(lambda e, tc=tc: e.tensor_copy(A_b[:, tc, :], A_f[:, tc, :]), rd=[tA[tc]], wr_=[tA[tc]])
        dv(lambda e, tc=tc: e.tensor_scalar(Wt_f[:, tc, :], oh1, wt1, None, ALU.mult), wr_=[tW[tc]])
        dv(lambda e, tc=tc: e.scalar_tensor_tensor(Wt_f[:, tc, :], oh2, wt2, Wt_f[:, tc, :], ALU.mult, ALU.add), rd=[tW[tc]], wr_=[tW[tc]])

    onesb = cx.sb("onesb", [128, 128], BF16)
    Lb = cx.sb("Lb", [128, 128], BF16)
    iof = cx.sb("iof", [128, CAP], F32)
    ioi = cx.sb("ioi", [128, CAP], I32)
    flags = cx.sb("flags", [1, NR * 32], I32)
    tcst, tflag = T(), T()
    P.op("pool", lambda e: e.memset(onesb[:], 1.0), writes=[tcst])
    P.op("pool", lambda e: e.iota(ioi[:], pattern=[[1, CAP]], base=0, channel_multiplier=-1), writes=[tcst])
    P.op("dve", lambda e: e.tensor_single_scalar(Lb[:], ioi[:], 0, ALU.is_gt), reads=[tcst], writes=[tcst])
    P.op("pool", lambda e: e.iota(ioi[:], pattern=[[1, CAP]], base=0, channel_multiplier=0), reads=[tcst], writes=[tcst])
    P.op("dve", lambda e: e.tensor_copy(iof[:], ioi[:]), reads=[tcst], writes=[tcst])
    for tc in range(NTC):
        for c2 in range(tc + 1):
            P.op("pe", lambda e, c2=c2, tc=tc: e.matmul(banks[7][:, 0:32], (Lb if c2 == tc else onesb)[:], A_b[:, c2, :],
                                                         start=(c2 == 0), stop=(c2 == tc)),
                 reads=[tA[c2], tcst], writes=[bankT[7]])
        P.op("dve", lambda e, tc=tc: e.tensor_tensor(rank_f[:, tc, :], banks[7][:, 0:32], A_f[:, tc, :], ALU.mult),
             reads=[bankT[7], tA[tc]], writes=[tR[tc]])
        P.op("dve", lambda e, tc=tc: e.scalar_tensor_tensor(rank_f[:, tc, :], A_f[:, tc, :], -1.0, rank_f[:, tc, :], ALU.add, ALU.add),
             reads=[tR[tc], tA[tc]], writes=[tR[tc]])
    for c2 in range(NTC):
        P.op("pe", lambda e, c2=c2: e.matmul(banks[7][0:1, 64:96], onesb[:, 0:1], A_b[:, c2, :], start=(c2 == 0), stop=(c2 == NTC - 1)),
             reads=[tA[c2], tcst], writes=[bankT[7]])
    for r in range(NR):
        P.op("dve", lambda e, r=r: e.tensor_single_scalar(flags[0:1, r * 32:(r + 1) * 32], banks[7][0:1, 64:96], r * CAP - 0.5, ALU.is_gt),
             reads=[bankT[7]], writes=[tflag])

    P.dma("sp", gbc[:], rows[4:5, :].to_broadcast([128, D]), sg, writes=[tgbc])

    selr = Ring(cx, "sel", [128, NTC, CAP], BF16, 2, dma=False)
    selTr = Ring(cx, "selT", [128, NTC * CAP], BF16, 2, dma=False)
    actr = Ring(cx, "actT", [128, 4, CAP], BF16, 1, dma=False)
    wslot = [1]

    def wload(src3, n_k):
        sl = SL[wslot[0] % NSLOT]
        wslot[0] += 1
        wv_ = sl.v.rearrange("p (k n) -> p k n", k=n_k)
        step = max(1, n_k // 4)
        for kk in range(0, n_k, step):
            P.dma("pool", wv_[:, kk:kk + step, :], src3[kk * 128:(kk + step) * 128, :].rearrange("(k p) n -> p k n", p=128),
                  sl.sem, writes=sl.wW())
        return wv_, sl

    st_ = {}
    W = {}
    ycnt = [0]

    def LW_gu(e):
        W[e] = dict(g=wload(wg_d[e], KD), u=wload(wu_d[e], KD))

    def LW_d(e):
        W[e]["d"] = wload(wd_d[e], 4)

    def G(e, r):
        sel, selT_, _ = selr.next()
        for c in range(NTC):
            P.op("dve", lambda en, sel=sel, c=c, e=e, r=r: en.tensor_scalar(sel[:, c, :], iof[:], rank_f[:, c, e:e + 1], -float(r * CAP),
                                                                             ALU.subtract, ALU.is_equal),
                 reads=[tR[c], tcst], writes=[selT_])
        xe, xeT, _ = xer.next()
        for k4 in range(4):
            bk = k4
            for kk in range(4):
                k = k4 * 4 + kk
                for c in range(NTC):
                    P.op("pe", lambda en, bk=bk, kk=kk, k=k, c=c, sel=sel: en.matmul(
                        banks[bk][:, kk * 128:(kk + 1) * 128], hb[:, c, k * 128:(k + 1) * 128], sel[:, c, :],
                        start=(c == 0), stop=(c == NTC - 1), skip_group_check=True),
                        reads=[thb[c], selT_], writes=[bankT[bk]])
            P.op("act", lambda en, bk=bk, k4=k4, xe=xe: en.copy(xe[:, k4 * 4:(k4 + 1) * 4, :], banks[bk][:, :].rearrange("p (k s) -> p k s", k=4)),
                 reads=[bankT[bk]], writes=[xeT])
        sT_, sTT, _ = selTr.next()
        psv = banks[6][:].bitcast(BF16)
        for c in range(NTC):
            P.op("pe", lambda en, c=c, sel=sel, psv=psv: en.transpose(psv[:, c * 128:(c + 1) * 128], sel[:, c, :], cx.idb[:]),
                 reads=[selT_, cx.tid], writes=[bankT[6]])
        P.op("act", lambda en, sT_=sT_, psv=psv: en.copy(sT_[:], psv), reads=[bankT[6]], writes=[sTT])
        st_[(e, r)] = dict(xe=xe, xeT=xeT, sT=sT_, sTT=sTT)

    def C(e, r):
        s = st_[(e, r)]
        for (key, bk) in (("g", 4), ("u", 5)):
            w, wS = W[e][key]
            for f in range(4):
                for k in range(KD):
                    P.op("pe", lambda en, w=w, bk=bk, f=f, k=k, xe=s["xe"]: en.matmul(
                        banks[bk][:, f * 128:(f + 1) * 128], w[:, k, f * 128:(f + 1) * 128], xe[:, k, :],
                        start=(k == 0), stop=(k == KD - 1), skip_group_check=True),
                        reads=wS.rW() + [s["xeT"]], writes=[bankT[bk]])
        sgl, sglT, _ = scr.next()
        act, actT_, _ = actr.next()
        P.op("act", lambda en, sgl=sgl: en.activation(sgl[:], banks[4][:, :], AF.Silu), reads=[bankT[4]], writes=[sglT])
        P.op("dve", lambda en, act=act, sgl=sgl: en.tensor_tensor(act[:].rearrange("p f s -> p (f s)"), sgl[:], banks[5][:, :], ALU.mult),
             reads=[sglT, bankT[5]], writes=[actT_])
        s["act"], s["actT"] = act, actT_

    def Dn(e, r):
        s = st_[(e, r)]
        wd, wdS = W[e]["d"]
        yi = ycnt[0] % 2
        ycnt[0] += 1
        yb = ybig[:, yi, :]
        for cb in range(4):
            for f in range(4):
                P.op("pe", lambda en, cb=cb, f=f, wd=wd, act=s["act"]: en.matmul(
                    banks[cb][:, :], act[:, f, :], wd[:, f, cb * 512:(cb + 1) * 512], start=(f == 0), stop=(f == 3)),
                    reads=[s["actT"]] + wdS.rW(), writes=[bankT[cb]])
            P.op("dve", lambda en, cb=cb, yb=yb: en.tensor_tensor(yb[:, cb * 512:(cb + 1) * 512], banks[cb][:, :], gbc[:, cb * 512:(cb + 1) * 512], ALU.mult),
                 reads=[bankT[cb], tgbc], writes=[ybT[yi], th2T])
        s["yb"], s["ybT"] = yb, ybT[yi]

    sc_i = [0]

    def S(e, r):
        s = st_[(e, r)]
        for c in range(NTC):
            for cb in range(4):
                bk = 6 + (sc_i[0] % 2)
                sc_i[0] += 1
                P.op("pe", lambda en, bk=bk, c=c, cb=cb, s=s: en.matmul(
                    banks[bk][:, :], s["sT"][:, c * 128:(c + 1) * 128], s["yb"][:, cb * 512:(cb + 1) * 512], start=True, stop=True),
                    reads=[s["sTT"], s["ybT"]], writes=[bankT[bk]])
                P.op("dve", lambda en, bk=bk, c=c, cb=cb, e=e: en.scalar_tensor_tensor(
                    xs[:, c, cb * 512:(cb + 1) * 512], banks[bk][:, :], Wt_f[:, c, e:e + 1], xs[:, c, cb * 512:(cb + 1) * 512],
                    ALU.mult, ALU.add), reads=[bankT[bk], tW[c], tx[c]], writes=[tx[c]])
        del st_[(e, r)]

    LW_gu(0)
    G(0, 0)
    C(0, 0)
    G(1, 0)
    for e in range(NE):
        LW_d(e)
        Dn(e, 0)
        S(e, 0)
        for r in range(1, NR):
            P.cond_begin(flags[0:1, r * 32 + e:r * 32 + e + 1], tflag)
            G(e, r)
            C(e, r)
            Dn(e, r)
            S(e, r)
            P.cond_end()
            xer.skip()
            selTr.skip()
        if e + 1 < NE:
            LW_gu(e + 1)
            C(e + 1, 0)
        if e + 2 < NE:
            G(e + 2, 0)

    so = P.dsem("so")
    for tc in range(NTC):
        P.dma("sp", xo[tc * 128:(tc + 1) * 128, :], xs[:, tc, :], so, reads=[tx[tc]])
    P.final_dsems = [so]
    return cx.finish()


def attn_outputs(r):
    o_all, rs_all = [], []
    for q in r:
        oT = np.asarray(q["oT"])
        od = oT.transpose(2, 0, 1).reshape(TOK, 1280)
        o_all.append(np.concatenate([od, np.asarray(q["on"])], axis=1))
        rs = np.asarray(q["rs"]).reshape(10, TOK).T
        rs_all.append(np.concatenate([rs, np.ones((TOK, 6), np.float32)], axis=1))
    return np.ascontiguousarray(np.concatenate(o_all, axis=0)), np.ascontiguousarray(np.concatenate(rs_all, axis=0))


def post_inputs(inp, l, x_full, o_full, mod, rs_full):
    m = mod[l].reshape(6, D)
    vT = colT(inp["mix_out_norm_g"][l])
    rows = np.ascontiguousarray(np.stack([m[2], inp["norm2_g"][l], m[4], m[3], m[5]]).astype(np.float32))
    wr = np.ascontiguousarray(np.concatenate([inp["router_group_w"][l], inp["router_expert_w"][l]], axis=1))
    rb = np.concatenate([inp["router_group_b"][l], inp["router_expert_b"][l]]).reshape(1, 36).astype(np.float32)
    maps = []
    for j in range(NCORES):
        sl = slice(j * TOK, (j + 1) * TOK)
        maps.append({"x": np.ascontiguousarray(x_full[sl]), "o": np.ascontiguousarray(o_full[sl]), "rs": np.ascontiguousarray(rs_full[sl]),
                     "vT": vT, "rows": rows,
                     "w_out": inp["w_out"][l], "wr": wr, "rb": rb,
                     "wg": inp["expert_w_gate"][l], "wu": inp["expert_w_up"][l], "wd": inp["expert_w_down"][l]})
    return maps


_PROGS = {}


def _prog(name, builder):
    if name not in _PROGS:
        _PROGS[name] = builder()
    return _PROGS[name]


def _run(nc, maps):
    res = run_bass_kernel_spmd(nc, maps, core_ids=list(range(NCORES)))
    return res.results


def kernel(**inp):
    inp = {k: np.asarray(v) for k, v in inp.items()}
    x = np.ascontiguousarray(inp["x"][0], dtype=np.float32)
    c = inp["c"].astype(np.float32)
    tabs = rope_tables()
    cT = np.ascontiguousarray(c.reshape(KD, 128).T)
    maps = []
    for j in range(NCORES):
        sl = slice(j * MODC, (j + 1) * MODC)
        maps.append({"cT": cT, "w": np.ascontiguousarray(inp["ada_w"][:, :, sl]),
                     "b": np.ascontiguousarray(inp["ada_b"][:, sl]).reshape(1, -1)})
    r = _run(_prog("ada", build_ada), maps)
    mod = np.concatenate([np.asarray(q["o"]).reshape(DEPTH, MODC) for q in r], axis=1)
    for l in range(DEPTH):
        r = _run(_prog("pre", build_pre), pre_inputs(inp, l, x, mod, tabs))
        qkv_all = np.concatenate([np.asarray(q["qkv"]) for q in r], axis=0)
        if qkv_all.dtype != NPBF:
            qkv_all = qkv_all.view(NPBF) if qkv_all.dtype.itemsize == 2 else qkv_all.astype(NPBF)
        r = _run(_prog("attn", build_attn), attn_inputs(inp, l, qkv_all))
        o_all, rs_all = attn_outputs(r)
        r = _run(_prog("post", build_post), post_inputs(inp, l, x, o_all, mod, rs_all))
        x = np.concatenate([np.asarray(q["xo"]) for q in r], axis=0)
    return np.ascontiguousarray(x[None].astype(np.float32))
```
